# Optimizing a Trainium2 kernel written in Bass

```python
import jax, jax.numpy as jnp
from jax import lax
import numpy as np

D_MODEL = 1024
BATCH = 16
SEQ = 2048
DEPTH = 4

BRANCH_W = D_MODEL // 2
MLSTM_HEADS = 4
MLSTM_DV = BRANCH_W // MLSTM_HEADS
MLSTM_DQK = MLSTM_DV // 2
MLSTM_CONV_K = 4
HGRN_HEADS = 4
HGRN_DK = BRANCH_W // HGRN_HEADS
HGRN_DV = BRANCH_W // HGRN_HEADS
CONV_CH = BRANCH_W
CONV_K = 31
N_BRANCH = 3
D_FF = -(-8 * D_MODEL // (3 * 256)) * 256
CHUNK = 64
EPS = 1e-6
NEG_BIG = -1e30
F_FLOOR = 1e-30
MLSTM_F_BIAS_LO = 3.0
MLSTM_F_BIAS_HI = 6.0

COL_SIZES = (
    2 * MLSTM_HEADS * MLSTM_DQK,
    MLSTM_HEADS * MLSTM_DV,
    MLSTM_HEADS * MLSTM_DV,
    MLSTM_HEADS,
    MLSTM_HEADS,
    HGRN_HEADS * HGRN_DK,
    HGRN_HEADS * HGRN_DK,
    HGRN_HEADS * HGRN_DV,
    HGRN_HEADS * HGRN_DV,
    CONV_CH,
    CONV_CH,
    N_BRANCH * D_MODEL,
)
D_IN = sum(COL_SIZES)

kernel_name = "hybrid_mlstm_hgrn2_conformer_griffin_merge"


def _col_offsets():
    offs = [0]
    for s in COL_SIZES:
        offs.append(offs[-1] + s)
    return offs


def rmsnorm(x, g):
    xf = x.astype(jnp.float32)
    y = xf * lax.rsqrt(jnp.mean(xf * xf, axis=-1, keepdims=True) + EPS)
    return (y * g.astype(jnp.float32)).astype(x.dtype)


def layernorm(x, g, b):
    xf = x.astype(jnp.float32)
    mu = jnp.mean(xf, axis=-1, keepdims=True)
    var = jnp.mean(jnp.square(xf - mu), axis=-1, keepdims=True)
    y = (xf - mu) * lax.rsqrt(var + EPS)
    return (y * g.astype(jnp.float32) + b.astype(jnp.float32)).astype(x.dtype)


def causal_depthwise_conv(x, w, b):
    k = w.shape[0]
    out = lax.conv_general_dilated(
        x, w.astype(x.dtype)[:, None, :], window_strides=(1,), padding=((k - 1, 0),),
        dimension_numbers=("NWC", "WIO", "NWC"), feature_group_count=x.shape[-1])
    return out + b.astype(x.dtype)


def _to_chunks(t):
    bsz, s, h, d = t.shape
    return t.reshape(bsz, s // CHUNK, CHUNK, h, d).transpose(1, 0, 3, 2, 4)


def _from_chunks(t):
    n, bsz, h, l, d = t.shape
    return t.transpose(1, 0, 3, 2, 4).reshape(bsz, n * l, h, d)


def mlstm_chunkwise(q, k, v, log_i, log_f):
    bsz, s, h, dk = q.shape
    dv = v.shape[-1]
    f32 = jnp.float32
    qc = _to_chunks(q.astype(f32) * (dk ** -0.5))
    kc = _to_chunks(k.astype(f32))
    vc = _to_chunks(v.astype(f32))
    ic = _to_chunks(log_i[..., None])[..., 0]
    fc = _to_chunks(log_f[..., None])[..., 0]
    causal = jnp.tril(jnp.ones((CHUNK, CHUNK), dtype=bool))

    def step(carry, inp):
        c_st, n_st, m_st = carry
        q_, k_, v_, li, lf = inp
        b = jnp.cumsum(lf, axis=-1)
        dlog = jnp.where(causal, b[..., :, None] - b[..., None, :] + li[..., None, :], NEG_BIG)
        g = b + m_st[..., None]
        m_t = jnp.maximum(g, jnp.max(dlog, axis=-1))
        w = jnp.where(causal, jnp.exp(dlog - m_t[..., None]), 0.0)
        inter = jnp.exp(g - m_t)
        sc = jnp.einsum("bhtd,bhsd->bhts", q_, k_) * w
        num = jnp.einsum("bhts,bhsv->bhtv", sc, v_) + inter[..., None] * jnp.einsum("bhtd,bhvd->bhtv", q_, c_st)
        den = jnp.sum(sc, axis=-1) + inter * jnp.einsum("bhtd,bhd->bht", q_, n_st)
        h_out = num / jnp.maximum(jnp.abs(den), jnp.exp(-m_t))[..., None]
        b_last = b[..., -1]
        dec = b_last[..., None] - b + li
        m_new = jnp.maximum(b_last + m_st, jnp.max(dec, axis=-1))
        ws = jnp.exp(dec - m_new[..., None])
        keep = jnp.exp(b_last + m_st - m_new)
        c_new = keep[..., None, None] * c_st + jnp.einsum("bhs,bhsv,bhsd->bhvd", ws, v_, k_)
        n_new = keep[..., None] * n_st + jnp.einsum("bhs,bhsd->bhd", ws, k_)
        return (c_new, n_new, m_new), h_out

    init = (jnp.zeros((bsz, h, dv, dk), f32), jnp.zeros((bsz, h, dk), f32), jnp.zeros((bsz, h), f32))
    _, hs = lax.scan(step, init, (qc, kc, vc, ic, fc))
    return _from_chunks(hs)


def hgrn2_chunkwise(q, k, v, log_f):
    bsz, s, h, dk = q.shape
    dv = v.shape[-1]
    qc = _to_chunks(q * (dk ** -0.5))
    kc = _to_chunks(k)
    vc = _to_chunks(v)
    fc = _to_chunks(log_f)
    causal = jnp.tril(jnp.ones((CHUNK, CHUNK), dtype=bool))[:, :, None]

    def step(s_st, inp):
        q_, k_, v_, lf = inp
        b = jnp.cumsum(lf, axis=2)
        diff = b[:, :, :, None, :] - b[:, :, None, :, :]
        decay = jnp.where(causal, jnp.exp(jnp.where(causal, diff, 0.0)), 0.0)
        a = jnp.einsum("bhtc,bhsc,bhtsc->bhts", q_, k_, decay)
        o = jnp.einsum("bhts,bhsv->bhtv", a, v_) + jnp.einsum("bhtc,bhcv->bhtv", q_ * jnp.exp(b), s_st)
        b_last = b[:, :, -1]
        s_new = jnp.exp(b_last)[..., None] * s_st + jnp.einsum(
            "bhsc,bhsv->bhcv", k_ * jnp.exp(b_last[:, :, None] - b), v_)
        return s_new, o

    _, os_ = lax.scan(step, jnp.zeros((bsz, h, dk, dv), jnp.float32), (qc, kc, vc, fc))
    return _from_chunks(os_)


def mlstm_branch(m_qk, m_v, m_o, m_i, m_f, conv_w, conv_b, norm_g):
    bsz, s, _ = m_qk.shape
    f32 = jnp.float32
    qk = jax.nn.silu(causal_depthwise_conv(m_qk, conv_w, conv_b))
    q, k = jnp.split(qk, 2, axis=-1)
    q = q.reshape(bsz, s, MLSTM_HEADS, MLSTM_DQK)
    k = k.reshape(bsz, s, MLSTM_HEADS, MLSTM_DQK)
    v = m_v.reshape(bsz, s, MLSTM_HEADS, MLSTM_DV)
    h = mlstm_chunkwise(q, k, v, m_i.astype(f32), jax.nn.log_sigmoid(m_f.astype(f32)))
    hn = rmsnorm(h, norm_g.reshape(MLSTM_HEADS, MLSTM_DV)).reshape(bsz, s, -1)
    return (jax.nn.sigmoid(m_o.astype(f32)) * hn).astype(m_o.dtype)


def hgrn2_branch(h_f, h_q, h_i, h_g, lower_bound, norm_g):
    bsz, s, _ = h_f.shape
    f32 = jnp.float32
    zf = h_f.astype(f32).reshape(bsz, s, HGRN_HEADS, HGRN_DK)
    lb = lower_bound.reshape(HGRN_HEADS, HGRN_DK)
    f = lb + (1.0 - lb) * jax.nn.sigmoid(zf)
    log_f = jnp.log(jnp.maximum(f, F_FLOOR))
    k = (1.0 - lb) * jax.nn.sigmoid(-zf)
    q = h_q.astype(f32).reshape(bsz, s, HGRN_HEADS, HGRN_DK)
    v = h_i.astype(f32).reshape(bsz, s, HGRN_HEADS, HGRN_DV)
    o = hgrn2_chunkwise(q, k, v, log_f)
    on = rmsnorm(o, norm_g.reshape(HGRN_HEADS, HGRN_DV)).reshape(bsz, s, -1)
    return (on * jax.nn.silu(h_g.astype(f32))).astype(h_g.dtype)


def conformer_conv_branch(c_a, c_b, conv_w, conv_b, ln_g, ln_b):
    u = c_a * jax.nn.sigmoid(c_b)
    u = causal_depthwise_conv(u, conv_w, conv_b)
    return jax.nn.silu(layernorm(u, ln_g, ln_b))


def setup_inputs(seed: int = 0) -> dict:
    key = jax.random.key(seed)
    ks = jax.random.split(key, 24)
    f32 = jnp.float32

    def nrm(k, shape, scale):
        return scale * jax.random.normal(k, shape, f32)

    offs = _col_offsets()
    f_lo = offs[4]
    b_in = nrm(ks[3], (DEPTH, D_IN), 0.02)
    b_in = b_in.at[:, f_lo:f_lo + MLSTM_HEADS].add(
        jnp.linspace(MLSTM_F_BIAS_LO, MLSTM_F_BIAS_HI, MLSTM_HEADS, dtype=f32))
    qk_w = 2 * MLSTM_HEADS * MLSTM_DQK
    return {
        "x": jax.random.normal(ks[0], (BATCH, SEQ, D_MODEL), f32),
        "norm_mix_g": 1.0 + nrm(ks[1], (DEPTH, D_MODEL), 0.02),
        "w_in": nrm(ks[2], (DEPTH, D_MODEL, D_IN), D_MODEL ** -0.5),
        "b_in": b_in,
        "mlstm_conv_w": nrm(ks[4], (DEPTH, MLSTM_CONV_K, qk_w), MLSTM_CONV_K ** -0.5),
        "mlstm_conv_b": nrm(ks[5], (DEPTH, qk_w), 0.02),
        "mlstm_norm_g": 1.0 + nrm(ks[6], (DEPTH, MLSTM_HEADS * MLSTM_DV), 0.02),
        "hgrn_lb_logits": nrm(ks[7], (DEPTH, HGRN_HEADS * HGRN_DK), 0.1),
        "hgrn_norm_g": 1.0 + nrm(ks[8], (DEPTH, HGRN_HEADS * HGRN_DV), 0.02),
        "conv_w": nrm(ks[9], (DEPTH, CONV_K, CONV_CH), CONV_K ** -0.5),
        "conv_b": nrm(ks[10], (DEPTH, CONV_CH), 0.02),
        "conv_ln_g": 1.0 + nrm(ks[11], (DEPTH, CONV_CH), 0.02),
        "conv_ln_b": nrm(ks[12], (DEPTH, CONV_CH), 0.02),
        "w_branch_a": nrm(ks[13], (DEPTH, BRANCH_W, D_MODEL), BRANCH_W ** -0.5),
        "w_branch_b": nrm(ks[14], (DEPTH, BRANCH_W, D_MODEL), BRANCH_W ** -0.5),
        "w_branch_c": nrm(ks[15], (DEPTH, CONV_CH, D_MODEL), CONV_CH ** -0.5),
        "w_out": nrm(ks[16], (DEPTH, D_MODEL, D_MODEL), D_MODEL ** -0.5),
        "norm_ffn_g": 1.0 + nrm(ks[17], (DEPTH, D_MODEL), 0.02),
        "w_ffn_in": nrm(ks[18], (DEPTH, D_MODEL, 2 * D_FF), D_MODEL ** -0.5),
        "w_ffn_out": nrm(ks[19], (DEPTH, D_FF, D_MODEL), D_FF ** -0.5),
        "final_norm_g": 1.0 + nrm(ks[20], (D_MODEL,), 0.02),
    }


def reference(x, norm_mix_g, w_in, b_in, mlstm_conv_w, mlstm_conv_b, mlstm_norm_g,
              hgrn_lb_logits, hgrn_norm_g, conv_w, conv_b, conv_ln_g, conv_ln_b,
              w_branch_a, w_branch_b, w_branch_c, w_out, norm_ffn_g, w_ffn_in,
              w_ffn_out, final_norm_g):
    offs = _col_offsets()
    lb_p = jax.nn.softmax(hgrn_lb_logits.astype(jnp.float32), axis=0)
    lower_bounds = jnp.cumsum(lb_p, axis=0) - lb_p[0]
    for l in range(DEPTH):
        h = rmsnorm(x, norm_mix_g[l])
        z = h @ w_in[l] + b_in[l]
        (m_qk, m_v, m_o, m_i, m_f, h_f, h_q, h_i, h_g,
         c_a, c_b, gate) = jnp.split(z, offs[1:-1], axis=-1)
        y_a = mlstm_branch(m_qk, m_v, m_o, m_i, m_f, mlstm_conv_w[l], mlstm_conv_b[l], mlstm_norm_g[l])
        y_b = hgrn2_branch(h_f, h_q, h_i, h_g, lower_bounds[l], hgrn_norm_g[l])
        y_c = conformer_conv_branch(c_a, c_b, conv_w[l], conv_b[l], conv_ln_g[l], conv_ln_b[l])
        g_a, g_b, g_c = jnp.split(jax.nn.sigmoid(gate), N_BRANCH, axis=-1)
        merged = (g_a * (y_a @ w_branch_a[l]) + g_b * (y_b @ w_branch_b[l])
                  + g_c * (y_c @ w_branch_c[l]))
        x = x + merged @ w_out[l]
        h2 = rmsnorm(x, norm_ffn_g[l])
        ffn_g, ffn_u = jnp.split(h2 @ w_ffn_in[l], 2, axis=-1)
        x = x + (jax.nn.silu(ffn_g) * ffn_u) @ w_ffn_out[l]
    return rmsnorm(x, final_norm_g)
```

```python
import numpy as np
from contextlib import ExitStack
import concourse.bass as bass
import concourse.mybir as mybir
from concourse.bass_utils import run_bass_kernel_spmd

F32 = mybir.dt.float32
BF16 = mybir.dt.bfloat16
AF = mybir.ActivationFunctionType
ALU = mybir.AluOpType
EPS = 1e-6
BW = 512
O_QK, O_V, O_O, O_I, O_F, O_HF, O_HQ, O_HI, O_HG, O_CA, O_CB, O_G = (
    0, 512, 1024, 1536, 1540, 1544, 2056, 2568, 3080, 3592, 4104, 4616)
CONVK = 31
SLOT = 1024
NSLOT = 12
PREFETCH = 2


class StopBuild(Exception):
    pass


class Buf:
    __slots__ = ("name", "w", "r", "excl")

    def __init__(self, name="", excl=False):
        self.name = name
        self.w = None
        self.r = []
        self.excl = excl


class Ins:
    __slots__ = ("eng", "idx", "emit", "dma", "deps", "raw", "sig", "val")

    def __init__(self, eng, idx, emit, dma):
        self.eng, self.idx, self.emit, self.dma = eng, idx, emit, dma
        self.deps = ()
        self.raw = ()
        self.sig = False
        self.val = 0


class T:
    __slots__ = ("ap", "bufs")

    def __init__(self, ap, bufs):
        self.ap = ap
        self.bufs = bufs if isinstance(bufs, list) else [bufs]

    def __getitem__(self, idx):
        return T(self.ap[idx], self.bufs)

    def bc(self, shape):
        return T(self.ap.to_broadcast(shape), self.bufs)

    def re(self, pat, **kw):
        return T(self.ap.rearrange(pat, **kw), self.bufs)

    def sub(self, idx, buf):
        return T(self.ap[idx], [buf])


def _bufs(*ts):
    out = []
    for t in ts:
        if isinstance(t, T):
            out.extend(t.bufs)
    return out


def _a(t):
    return t.ap if isinstance(t, T) else t


class Prog:
    ENG = ("pe", "act", "dve", "pool", "sp")

    def __init__(self, nc):
        self.nc = nc
        self.q = {e: [] for e in self.ENG}
        self.fence_deps = []
        self.arena_dmas = []
        self.dma_count = {}

    def add(self, eng, emit, reads=(), writes=(), dma=None, arena=False):
        ins = Ins(eng, len(self.q[eng]), emit, dma)
        deps = set()
        raw = set()
        for b in reads:
            if b.w is not None:
                deps.add(b.w)
                raw.add(b.w)
            if b.excl:
                deps.update(r for r in b.r if r.eng != eng)
        for b in writes:
            if b.w is not None:
                deps.add(b.w)
            deps.update(b.r)
        deps.update(self.fence_deps)
        deps.discard(ins)
        ins.deps = deps
        ins.raw = raw
        for b in reads:
            b.r.append(ins)
        for b in writes:
            b.w = ins
            b.r = []
        if dma is not None:
            self.dma_count[dma] = self.dma_count.get(dma, 0) + 1
            ins.val = 16 * self.dma_count[dma]
            if arena:
                self.arena_dmas.append(ins)
        self.q[eng].append(ins)
        return ins

    def fence(self):
        deps = [self.q[e][-1] for e in self.ENG if self.q[e]]
        deps.extend(self.arena_dmas)
        self.arena_dmas = []
        self.fence_deps = deps

    def mm(self, out, lhsT, rhs, start=True, stop=True):
        o, l, r = out.ap, lhsT.ap, rhs.ap
        self.add("pe", lambda e: e.matmul(o, l, r, start=start, stop=stop),
                 reads=_bufs(lhsT, rhs) + ([] if start else _bufs(out)), writes=_bufs(out))

    def tr(self, out, in_, ident):
        o, i, d = out.ap, in_.ap, ident.ap
        self.add("pe", lambda e: e.transpose(o, i, d), reads=_bufs(in_, ident), writes=_bufs(out))

    def act(self, out, in_, func, bias=None, scale=None, eng="act"):
        kw = {}
        if bias is not None:
            kw["bias"] = _a(bias)
        if scale is not None:
            kw["scale"] = _a(scale)
        o, i = out.ap, in_.ap
        self.add("act", lambda e: e.activation(o, i, func, **kw),
                 reads=_bufs(in_, bias, scale), writes=_bufs(out))

    def tt(self, eng, out, in0, in1, op):
        o, a, b = out.ap, in0.ap, in1.ap
        self.add(eng, lambda e: e.tensor_tensor(o, a, b, op), reads=_bufs(in0, in1), writes=_bufs(out))

    def ts(self, eng, out, in0, s1, s2, op0, op1=None):
        o, a = out.ap, in0.ap
        s1a, s2a = _a(s1), _a(s2)
        if op1 is None:
            self.add(eng, lambda e: e.tensor_scalar(o, a, s1a, None, op0),
                     reads=_bufs(in0, s1), writes=_bufs(out))
        else:
            self.add(eng, lambda e: e.tensor_scalar(o, a, s1a, s2a, op0, op1),
                     reads=_bufs(in0, s1, s2), writes=_bufs(out))

    def stt(self, out, in0, scalar, in1, op0, op1):
        o, a, b = out.ap, in0.ap, in1.ap
        sa = _a(scalar)
        self.add("dve", lambda e: e.scalar_tensor_tensor(o, a, sa, b, op0, op1),
                 reads=_bufs(in0, scalar, in1), writes=_bufs(out))

    def scan(self, out, d0, d1, init, op0, op1):
        o, a, b = out.ap, d0.ap, d1.ap
        ia = _a(init)
        self.add("dve", lambda e: e.tensor_tensor_scan(o, a, b, ia, op0, op1),
                 reads=_bufs(d0, d1, init), writes=_bufs(out))

    def recip(self, out, in_):
        o, i = out.ap, in_.ap
        self.add("dve", lambda e: e.reciprocal(o, i), reads=_bufs(in_), writes=_bufs(out))

    def copy(self, eng, out, in_):
        o, i = out.ap, in_.ap
        if eng == "act":
            self.add("act", lambda e: e.activation(o, i, AF.Identity), reads=_bufs(in_), writes=_bufs(out))
        else:
            self.add(eng, lambda e: e.tensor_copy(o, i), reads=_bufs(in_), writes=_bufs(out))

    def memset(self, eng, out, val):
        o = out.ap
        self.add(eng, lambda e: e.memset(o, val), writes=_bufs(out))

    def dma(self, queue, out, in_, key, arena=False):
        o, i = _a(out), _a(in_)
        return self.add(queue, lambda e: e.dma_start(out=o, in_=i), reads=_bufs(in_),
                        writes=_bufs(out), dma=key, arena=arena)

    def emit_all(self, stack):
        nc = self.nc
        allq = [i for e in self.ENG for i in self.q[e]]
        for ins in allq:
            for d in ins.deps:
                if d.dma is None:
                    if d.eng == ins.eng and (d.eng == "pe" or ins.idx - d.idx > 2):
                        continue
                    d.sig = True
        for e in self.ENG:
            c = 0
            for ins in self.q[e]:
                if ins.dma is None and ins.sig:
                    c += 1
                    ins.val = c
        esem = {e: stack.enter_context(nc.semaphore("sem_" + e)) for e in self.ENG}
        dsem = {k: stack.enter_context(nc.semaphore("dsem_" + k)) for k in self.dma_count}
        engobj = {"pe": "tensor", "act": "scalar", "dve": "vector", "pool": "gpsimd", "sp": "sync"}

        def run(en, e):
            waited = {}
            for ins in self.q[en]:
                need = {}
                for d in ins.deps:
                    if d.dma is not None:
                        k, v = ("d", d.dma), d.val
                    else:
                        if d.eng == en and (en == "pe" or ins.idx - d.idx > 2):
                            continue
                        k, v = ("e", d.eng), d.val
                    if v > need.get(k, 0):
                        need[k] = v
                for k, v in need.items():
                    if waited.get(k, 0) >= v:
                        continue
                    waited[k] = v
                    e.wait_ge(dsem[k[1]] if k[0] == "d" else esem[k[1]], v)
                r = ins.emit(e)
                if r is None:
                    continue
                if ins.dma is not None:
                    r.then_inc(dsem[ins.dma], 16)
                elif ins.sig:
                    r.then_inc(esem[en], 1)

        block = stack.enter_context(nc.Block())

        @block.tensor
        def _(e):
            run("pe", e)

        @block.scalar
        def _(e):
            run("act", e)

        @block.vector
        def _(e):
            run("dve", e)

        @block.gpsimd
        def _(e):
            run("pool", e)

        @block.sync
        def _(e):
            run("sp", e)


def param_layout(cfg):
    D = cfg["D"]
    KD = D // 128
    ent = [("bin_a", 12), ("bin_i", 1), ("bin_f", 1), ("bin_b", (3072 + 3 * D) // 128),
           ("nmix", KD), ("nffn", KD), ("mcw", 16), ("mcb", 4), ("mng", 4), ("lbl", 4),
           ("hng", 4), ("cw", CONVK * 4), ("cb", 4), ("lng", 4), ("lnb", 4)]
    off, o = {}, 0
    for k, n in ent:
        off[k] = o
        o += n
    return off, o


def pack_params(cfg, inp):
    D, L = cfg["D"], cfg["DEPTH"]
    rows = []
    for l in range(L):
        b = inp["b_in"][l]
        pad = np.zeros(124, np.float32)
        rows += [b[0:1536], np.concatenate([b[1536:1540], pad]), np.concatenate([b[1540:1544], pad]),
                 b[1544:], inp["norm_mix_g"][l], inp["norm_ffn_g"][l], inp["mlstm_conv_w"][l].reshape(-1),
                 inp["mlstm_conv_b"][l], inp["mlstm_norm_g"][l], inp["hgrn_lb_logits"][l],
                 inp["hgrn_norm_g"][l], inp["conv_w"][l].reshape(-1), inp["conv_b"][l],
                 inp["conv_ln_g"][l], inp["conv_ln_b"][l]]
    rows.append(inp["final_norm_g"])
    flat = np.concatenate([np.asarray(r, np.float32).reshape(-1) for r in rows])
    n = flat.size // 128
    npad = -(-n // 128) * 128
    out = np.zeros((npad, 128), np.float32)
    out.reshape(-1)[:flat.size] = flat
    return out


def make_consts():
    c = np.zeros((128, 896), np.float32)
    c[:, 0:128] = np.eye(128, dtype=np.float32)
    s = np.arange(128)
    c[:, 128:256] = (s[:, None] <= s[None, :]).astype(np.float32)
    c[:, 256:384] = ((s[:, None] <= s[None, :]) & ((s[:, None] // 64) == (s[None, :] // 64))).astype(np.float32)
    for h in range(4):
        c[h, 384 + h * 128: 384 + (h + 1) * 128] = 1.0
    return c


def build(cfg):
    D, TSEQ, TU, DEPTH, DFF, NSEQ = cfg["D"], cfg["T"], cfg["TU"], cfg["DEPTH"], cfg["DFF"], cfg["NSEQ"]
    KD, KF, NU = D // 128, DFF // 128, TSEQ // TU
    TN = min(512, TU)
    NT, NTT = TU // TN, TU // 128
    DIN = O_G + 3 * D
    poff, RPL = param_layout(cfg)
    RP = -(-(DEPTH * RPL + KD) // 128) * 128
    KFH = KF // 2

    nc = bass.Bass("TRN2", target_bir_lowering=False)
    P = Prog(nc)
    stack = ExitStack()

    def dram(name, shape, kind="ExternalInput"):
        return nc.dram_tensor(name, list(shape), F32, kind=kind).ap()

    x_d = dram("x", [NSEQ, TSEQ, D])
    out_d = dram("out", [NSEQ, TSEQ, D], "ExternalOutput")
    win_d = dram("w_in", [DEPTH, D, DIN])
    bin_d = dram("b_in", [DEPTH, DIN])
    wa_d = dram("w_branch_a", [DEPTH, BW, D])
    wb_d = dram("w_branch_b", [DEPTH, BW, D])
    wc_d = dram("w_branch_c", [DEPTH, BW, D])
    wo_d = dram("w_out", [DEPTH, D, D])
    wfi_d = dram("w_ffn_in", [DEPTH, D, 2 * DFF])
    wfo_d = dram("w_ffn_out", [DEPTH, DFF, D])
    par_d = dram("params", [RP, 128])
    con_d = dram("consts", [128, 896])

    def sb(name, shape, dt):
        return stack.enter_context(nc.sbuf_tensor(name, list(shape), dt))

    def tile(name, shape, dt):
        return T(sb(name, shape, dt)[:], Buf(name))

    xT_t = sb("xT", [128, KD, TSEQ], F32)
    xT = [[T(xT_t[:, m, u * TU:(u + 1) * TU], Buf("x")) for u in range(NU)] for m in range(KD)]
    hT_t = sb("hT", [128, KD, TU], BF16)
    hT = [T(hT_t[:, :, n * TN:(n + 1) * TN], Buf("h")) for n in range(NT)]
    ring_t = sb("ring", [128, NSLOT * SLOT], BF16)
    ring_bufs = [Buf("ring") for _ in range(NSLOT)]
    pcols = tile("pcols", [128, RP], F32)
    identf = tile("identf", [128, 128], F32)
    identb = tile("identb", [128, 128], BF16)
    onesb = tile("onesb", [128, 128], BF16)
    maskc = tile("maskc", [128, 128], BF16)
    maskbd = tile("maskbd", [128, 128], BF16)
    sel = tile("sel", [4, 512], F32)
    lbt = tile("lbt", [128, DEPTH, 4], F32)
    omlt = tile("omlt", [128, DEPTH, 4], F32)
    nomlt = tile("nomlt", [128, DEPTH, 4], F32)
    vb = tile("vb", [128, 2, 512], F32)
    Saug = tile("Saug", [128, 2, 256], F32)
    Saugb = tile("Saugb", [128, 2, 256], BF16)
    Sh = tile("Sh", [128, 4, 128], F32)
    Shb = tile("Shb", [128, 4, 128], BF16)
    Shbr_t = sb("Shbr", [128, 4, 3, 128], BF16)
    Shbr = [[T(Shbr_t[:, h, k, :], Buf("shbr")) for k in range(3)] for h in range(4)]
    hc = [0, 0, 0, 0]
    carry = tile("carry", [4, 4], F32)
    qkpad = tile("qkpad", [128, 4, 4], BF16)
    upad = tile("upad", [128, 4, CONVK - 1], BF16)
    zero1 = tile("zero1", [128, 1], F32)
    AE = cfg["ARENA"]
    arena_t = sb("arena", [128, AE], BF16)
    ps_t = [stack.enter_context(nc.psum_tensor("ps%d" % i, [128, 512], F32)) for i in range(8)]
    ps_bufs = [Buf("ps", excl=True) for _ in range(8)]
    st = {"ps": 0, "ar": 0}

    def PS(shape=None, dt=F32):
        i = st["ps"]
        st["ps"] = (i + 1) % 8
        ap = ps_t[i][:]
        if dt == BF16:
            ap = ap.bitcast(BF16)
        return T(ap, ps_bufs[i])

    def AR(shape, dt, parts=128):
        n = 1
        for s_ in shape:
            n *= s_
        ne = n * (2 if dt == F32 else 1)
        off = st["ar"]
        off += off % 2
        assert off + ne <= AE, ("arena overflow", off, ne, AE)
        st["ar"] = off + ne
        ap = arena_t[:, off:off + ne]
        if dt == F32:
            ap = ap.bitcast(F32)
        if len(shape) == 2:
            ap = ap.rearrange("p (a b) -> p a b", b=shape[1])
        elif len(shape) == 3:
            ap = ap.rearrange("p (a b c) -> p a b c", b=shape[1], c=shape[2])
        if parts != 128:
            ap = ap[0:parts]
        return T(ap, Buf("ar"))

    def phase():
        P.fence()
        st["ar"] = 0

    pieces = []
    rst = {"issued": 0, "ptr": 0, "next": 0, "tiles": {}, "slots": {}, "held": []}

    def piece_list(l):
        pl = []

        def win(c0, ncol):
            pl.append((win_d[l, :, c0:c0 + ncol].rearrange("(k p) c -> p k c", p=128), KD, ncol))
        win(O_QK, 512); win(O_V, 512); win(O_O, 512); win(O_I, 8)
        win(O_HI, 512); win(O_HG, 512)
        for h in range(4):
            win(O_HF + 128 * h, 128); win(O_HQ + 128 * h, 128)
        win(O_CB, 512); win(O_CA, 512)
        for m in range(KD):
            for g in range(3):
                win(O_G + g * D + m * 128, 128)
            for wd in (wa_d, wb_d, wc_d):
                pl.append((wd[l, :, m * 128:(m + 1) * 128].rearrange("(k p) c -> p k c", p=128), 4, 128))
        for c0 in range(0, D, 512):
            ncol = min(512, D - c0)
            pl.append((wo_d[l, :, c0:c0 + ncol].rearrange("(k p) c -> p k c", p=128), KD, ncol))
        for j in range(0, KF, 2):
            pl.append((wfi_d[l, :, j * 128:(j + 2) * 128].rearrange("(k p) c -> p k c", p=128), KD, 256))
            pl.append((wfi_d[l, :, DFF + j * 128:DFF + (j + 2) * 128].rearrange("(k p) c -> p k c", p=128), KD, 256))
        for c0 in range(0, D, 256):
            for hf in range(2):
                pl.append((wfo_d[l, hf * KFH * 128:(hf + 1) * KFH * 128, c0:c0 + 256]
                           .rearrange("(k p) c -> p k c", p=128), KFH, 256))
        return pl

    def ring_issue():
        i = rst["issued"]
        src, kt, ncol = pieces[i]
        ns = -(-(kt * ncol) // SLOT)
        live = list(rst["slots"].values())

        def free(p):
            return p + ns <= NSLOT and all(p + ns <= a or p >= a + n for a, n in live)
        cand = [p for p in list(range(rst["ptr"], NSLOT)) + list(range(0, rst["ptr"])) if free(p)]
        if not cand:
            return False
        p0 = cand[0]
        rst["ptr"] = (p0 + ns) % NSLOT
        rst["slots"][i] = (p0, ns)
        ap = ring_t[:, p0 * SLOT:p0 * SLOT + kt * ncol].rearrange("p (k c) -> p k c", c=ncol)
        t = T(ap, ring_bufs[p0:p0 + ns])
        P.dma("pool", t, src, "ring%d" % p0)
        rst["tiles"][i] = t
        rst["issued"] = i + 1
        return True

    def W(hold=False):
        i = rst["next"]
        if not hold:
            for k in rst["held"]:
                rst["slots"].pop(k)
            rst["held"] = []
        while rst["issued"] < min(len(pieces), i + 1 + PREFETCH):
            if not ring_issue():
                assert rst["issued"] > i, "ring too small for held pieces"
                break
        rst["next"] = i + 1
        rst["held"].append(i)
        return rst["tiles"].pop(i)

    for s_ in range(NSEQ):
        for l in range(DEPTH):
            for u in range(NU):
                pieces.extend(piece_list(l))

    def pc(l, name, j=0, parts=128):
        c = l * RPL + poff[name] + j
        return pcols[0:parts, c:c + 1]

    cst = AR([896], F32)
    P.dma("sp", cst, con_d[:, :], "cst", arena=True)
    P.copy("dve", identf, cst[:, 0:128])
    P.copy("dve", identb, cst[:, 0:128])
    P.copy("dve", maskc, cst[:, 128:256])
    P.copy("dve", maskbd, cst[:, 256:384])
    P.copy("dve", sel, cst[0:4, 384:896])
    P.memset("dve", onesb, 1.0)
    P.memset("dve", zero1, 0.0)
    DBG = cfg.get("DBG", 0)
    for r0 in range(0, RP if not DBG & 2 else 0, 128):
        stg = AR([128], F32)
        P.dma("sp", stg, par_d[r0:r0 + 128, :], "par%d" % (r0 // 128), arena=True)
        pt = PS()
        P.tr(pt[:, 0:128], stg, identf)
        P.copy("act", pcols[:, r0:r0 + 128], pt[:, 0:128])
    ex = AR([DEPTH, 4], F32)
    for l in range(DEPTH if not DBG & 1 else 0):
        P.act(ex[:, l, :], pcols[:, l * RPL + poff["lbl"]:l * RPL + poff["lbl"] + 4], AF.Exp)
    sm = AR([4], F32)
    if DBG & 1:
        P.memset("dve", ex, 1.0)
    P.copy("dve", sm, ex[:, 0, :])
    for l in range(1, DEPTH):
        P.tt("dve", sm, sm, ex[:, l, :], ALU.add)
    P.recip(sm, sm)
    P.memset("dve", lbt[:, 0, :], 0.0)
    for l in range(1, DEPTH):
        pl_ = AR([4], F32)
        P.tt("dve", pl_, ex[:, l, :], sm, ALU.mult)
        P.tt("dve", lbt[:, l, :], lbt[:, l - 1, :], pl_, ALU.add)
    P.ts("dve", omlt, lbt, -1.0, 1.0, ALU.mult, ALU.add)
    P.ts("dve", nomlt, omlt, -1.0, None, ALU.mult)

    def rmsnorm_to(dst_fn, xs, gcol_fn, u):
        for n in range(NT):
            acc = PS()
            for m in range(KD):
                sq = AR([TN], BF16) if False else sqbuf[m % 2]
                P.act(sq, xs[m][u][:, n * TN:(n + 1) * TN], AF.Square)
                P.mm(acc[:, 0:TN], onesb, sq, start=(m == 0), stop=(m == KD - 1))
            sd = rsbuf
            P.act(sd, acc[:, 0:TN], AF.Sqrt, bias=epsD, scale=1.0 / D)
            P.recip(sd, sd)
            dst_fn(n, sd)

    def group_norm_gate(src, gcol, gate_inout, width, sq, sd):
        P.act(sq, src, AF.Square)
        acc = PS()
        P.mm(acc[:, 0:width], onesb, sq)
        P.act(sd, acc[:, 0:width], AF.Sqrt, bias=eps128, scale=1.0 / 128)
        P.recip(sd, sd)
        P.stt(sd, src, gcol, sd, ALU.mult, ALU.mult)
        P.tt("dve", gate_inout, sd, gate_inout, ALU.mult)

    def conv_fm(dst_evac, src_pad, K, wname, l, diags):
        for j in range(4):
            diag = diags[j % len(diags)]
            for k in range(K):
                P.ts("dve", diag[k], identb, pc(l, wname, k * 4 + j), None, ALU.mult)
            if K > 4:
                chk(5.17 + 0.0001 * (1 + 10 * j))
            for n in range(NT):
                acc = PS()
                for k in range(K):
                    P.mm(acc[:, 0:TN], diag[k], src_pad[:, j, n * TN + k:n * TN + k + TN],
                         start=(k == 0), stop=(k == K - 1))
                if K > 4:
                    chk(5.17 + 0.0001 * (2 + 10 * j + 3 * n))
                dst_evac(j, n, acc)
                if K > 4:
                    chk(5.17 + 0.0001 * (3 + 10 * j + 3 * n))

    eps_t = tile("eps_t", [128, 2], F32)
    P.memset("dve", eps_t[:, 0:1], EPS)
    P.memset("dve", eps_t[:, 1:2], EPS)
    epsD = eps_t[:, 0:1]
    eps128 = eps_t[:, 1:2]
    sqbuf = [tile("sqb%d" % i, [128, TN], BF16) for i in range(2)]
    rsbuf = tile("rsbuf", [128, TN], F32)

    out_bufs = []

    ones1 = tile("ones1", [128, 1], F32)
    P.memset("dve", ones1, 1.0)

    def bmid(t, n):
        return T(t.ap.unsqueeze(1).to_broadcast([t.ap.shape[0], n, t.ap.shape[1]]), t.bufs)

    def blast(t, n):
        return T(t.ap.unsqueeze(2).to_broadcast([t.ap.shape[0], t.ap.shape[1], n]), t.bufs)

    def PR(h):
        return slice(64 * (h % 2), 64 * (h % 2) + 64)

    def NS(n):
        return slice(n * TN, (n + 1) * TN)

    def proj_fm(w, j, n, ncol=128):
        acc = PS()
        for k in range(KD):
            P.mm(acc[:, 0:TN], w[:, k, j * ncol:(j + 1) * ncol], hT[n][:, k, :], start=(k == 0), stop=(k == KD - 1))
        return acc

    NC = NTT
    NCH = TU // 64
    HSC = 128.0 ** -0.5

    STOP = cfg.get("STOP", 99)

    def chk(k):
        if STOP <= k:
            raise StopBuild()

    for s in range(NSEQ):
        phase()
        xstg = [AR([D], F32), AR([D], F32)]
        for tt_ in range(TSEQ // 128):
            stg = xstg[tt_ % 2]
            P.dma("sp", stg, x_d[s, tt_ * 128:(tt_ + 1) * 128, :], "xin%d" % (tt_ % 2), arena=True)
            u_, c0 = divmod(tt_ * 128, TU)
            for m0 in range(0, KD if not DBG & 8 else 0, 4):
                pt = PS()
                mm_ = min(4, KD - m0)
                for i in range(mm_):
                    P.tr(pt[:, i * 128:(i + 1) * 128], stg[:, (m0 + i) * 128:(m0 + i + 1) * 128], identf)
                for i in range(mm_ if not DBG & 128 else 0):
                    P.copy("dve" if DBG & 32 else ("act" if i % 2 else "dve"), xT[m0 + i][u_][:, c0:c0 + 128], pt[:, i * 128:(i + 1) * 128])

        try:
            for l in range(DEPTH if STOP > 0 else 0):
                P.memset("dve", Saug, 0.0)
                P.memset("dve", Saugb, 0.0)
                P.memset("dve", Sh, 0.0)
                P.memset("dve", Shb, 0.0)
                for h_ in range(4):
                    hc[h_] = 0
                    P.memset("dve", Shbr[h_][0], 0.0)
                P.memset("dve", carry, 0.0)
                P.memset("dve", qkpad, 0.0)
                P.memset("dve", upad, 0.0)
                P.dma("sp", vb[:, 0, :], bin_d[l:l + 1, O_V:O_V + 512].to_broadcast([128, 512]), "vb0")
                P.dma("sp", vb[:, 1, :], bin_d[l:l + 1, O_HI:O_HI + 512].to_broadcast([128, 512]), "vb1")

                for u in range(NU):
                    phase()
                    ya = AR([4, TU], BF16)
                    yb = AR([4, TU], BF16)
                    yc = AR([4, TU], BF16)
                    YRES = st["ar"]

                    def mk_h(n, sd):
                        for m in range(KD):
                            P.stt(hT[n][:, m, :], xT[m][u][:, NS(n)], pc(l, "nmix", m), sd, ALU.mult, ALU.mult)
                    rmsnorm_to(mk_h, xT, None, u)
                    chk(1)

                    qk = AR([4, TU], BF16)
                    vtok = AR([NTT, 512], BF16)
                    mark_m = st["ar"]
                    qkp = AR([4, 3 + TU], BF16)
                    P.copy("pool", qkp[:, :, 0:3], qkpad[:, :, 0:3])
                    w = W()
                    for j in range(4):
                        for n in range(NT):
                            acc = proj_fm(w, j, n)
                            P.act(qkp[:, j, 3 + n * TN:3 + (n + 1) * TN], acc[:, 0:TN], AF.Identity, bias=pc(l, "bin_a", j))
                    P.copy("pool", qkpad[:, :, 0:3], qkp[:, :, TU:TU + 3])

                    def ev_qk(j, n, acc):
                        P.act(qk[:, j, NS(n)], acc[:, 0:TN], AF.Silu, bias=pc(l, "mcb", j))
                    conv_fm(ev_qk, qkp, 4, "mcw", l, [[AR([128], BF16) for _ in range(4)] for _ in range(2)])
                    for j in range(2):
                        P.ts("dve", qk[:, j, :], qk[:, j, :], 0.125, None, ALU.mult)
                    P.fence()
                    st["ar"] = mark_m
                    chk(2)
                    w = W()
                    for t_ in range(NTT):
                        acc = PS()
                        n, c0 = divmod(t_ * 128, TN)
                        for k in range(KD):
                            P.mm(acc, hT[n][:, k, c0:c0 + 128], w[:, k, :], start=(k == 0), stop=(k == KD - 1))
                        P.tt("dve", vtok[:, t_, :], acc, vb[:, 0, :], ALU.add)
                    w = W()
                    for j in range(4):
                        for n in range(NT):
                            acc = proj_fm(w, j, n)
                            P.act(ya[:, j, NS(n)], acc[:, 0:TN], AF.Sigmoid, bias=pc(l, "bin_a", 8 + j))
                    w = W()
                    R0 = AR([TU], F32, parts=4)
                    R1 = AR([TU], F32, parts=4)
                    R2 = AR([TU], F32, parts=4)
                    R3 = AR([TU], F32, parts=4)
                    nb = AR([1], F32, parts=4)
                    P.ts("dve", nb, pc(l, "bin_f", 0, 4), -1.0, None, ALU.mult)
                    onesrow = ones1[0:4, :].bc([4, TN])
                    for n in range(NT):
                        sl = NS(n)
                        af = PS()
                        ai = PS()
                        for k in range(KD):
                            P.mm(af[0:4, 0:TN], w[:, k, 4:8], hT[n][:, k, :], start=(k == 0), stop=(k == KD - 1))
                        for k in range(KD):
                            P.mm(ai[0:4, 0:TN], w[:, k, 0:4], hT[n][:, k, :], start=(k == 0), stop=(k == KD - 1))
                        P.act(R0[:, sl], af[0:4, 0:TN], AF.Exp, bias=nb, scale=-1.0)
                        P.act(R0[:, sl], R0[:, sl], AF.Ln, bias=1.0)
                        init = carry[:, 0:1] if n == 0 else R1[:, n * TN - 1:n * TN]
                        P.scan(R1[:, sl], onesrow, R0[:, sl], init, ALU.mult, ALU.subtract)
                        P.stt(R0[:, sl], ai[0:4, 0:TN], pc(l, "bin_i", 0, 4), R1[:, sl], ALU.add, ALU.subtract)
                        initm = carry[:, 1:2] if n == 0 else R2[:, n * TN - 1:n * TN]
                        P.scan(R2[:, sl], R0[:, sl], R0[:, sl], initm, ALU.max, ALU.max)
                    dec = AR([NC], F32, parts=4)
                    P.tt("dve", dec[:, 0:1], carry[:, 1:2], R2[:, 127:128], ALU.subtract)
                    if NC > 1:
                        P.tt("dve", dec[:, 1:NC], R2[:, 127:TU - 128:128], R2[:, 255:TU:128], ALU.subtract)
                    P.act(dec, dec, AF.Exp)
                    P.copy("dve", carry[:, 0:1], R1[:, TU - 1:TU])
                    P.copy("dve", carry[:, 1:2], R2[:, TU - 1:TU])
                    for c in range(NC):
                        cs = slice(c * 128, (c + 1) * 128)
                        me = R2[:, c * 128 + 127:c * 128 + 128]
                        P.ts("dve", R3[:, cs], R0[:, cs], me, None, ALU.subtract)
                        P.ts("dve", R1[:, cs], R1[:, cs], me, -1.0, ALU.add, ALU.mult)
                    P.act(R3, R3, AF.Exp)
                    P.act(R1, R1, AF.Exp)
                    wcol = AR([NC, 4], F32)
                    for c in range(NC):
                        pt = PS()
                        P.tr(pt[:, 0:4], R3[:, c * 128:(c + 1) * 128], identf[0:4, 0:4])
                        P.copy("act", wcol[:, c, :], pt[:, 0:4])
                    decb = AR([4, NC], F32)
                    pt = PS()
                    for h in range(4):
                        P.mm(pt[:, h * NC:(h + 1) * NC], sel[:, h * 128:(h + 1) * 128], dec)
                    P.copy("act", decb.re("p h c -> p (h c)"), pt[:, 0:4 * NC])
                    for h in range(4):
                        P.ts("dve", Saug[PR(h), h // 2, :], Saug[PR(h), h // 2, :], decb[PR(h), h, 0:1], None, ALU.mult)
                    P.copy("act", Saugb, Saug)

                    pm = AR([4, 128], BF16)
                    chk(3)
                    ktok = AR([256], BF16)
                    vaug = AR([4, 256], BF16)
                    dmx = AR([512], F32)
                    hh = AR([512], F32)
                    sq = AR([512], BF16)
                    sd = AR([512], F32)
                    HO = (0, 2, 1, 3)
                    for c in range(NC):
                        cs = slice(c * 128, (c + 1) * 128)
                        scb = [PS(), PS()]
                        for pos, h in enumerate(HO):
                            P.mm(scb[pos // 2][:, (pos % 2) * 128:(pos % 2) * 128 + 128], qk[PR(h), 2 + h // 2, cs], qk[PR(h), h // 2, cs])
                        for par in range(2):
                            P.tt("dve", pm[:, 2 * par:2 * par + 2, :], scb[par][:, 0:256].re("p (g t) -> p g t", g=2),
                                 bmid(maskc, 2), ALU.mult)
                        chk(3.1)
                        kt_ps = PS(dt=BF16)
                        for j in range(2):
                            P.tr(kt_ps[:, j * 128:(j + 1) * 128], qk[:, 2 + j, cs], identb)
                        P.copy("act", ktok, kt_ps[:, 0:256])
                        chk(3.2)
                        wc_ = wcol[:, c, :]
                        P.tt("pool", vaug[:, :, 0:128], vtok[:, c, :].re("p (h v) -> p h v", h=4), blast(wc_, 128), ALU.mult)
                        P.copy("pool", vaug[:, :, 128:256], blast(wc_, 128))
                        chk(3.3)
                        numb = [PS(), PS()]
                        denb = [PS(), PS()]
                        for pos, h in enumerate(HO):
                            ps_ = slice((pos % 2) * 128, (pos % 2) * 128 + 128)
                            nb_, db_ = numb[pos // 2], denb[pos // 2]
                            P.mm(nb_[:, ps_], vaug[:, h, 0:128], pm[:, pos, :], start=True, stop=False)
                            P.mm(nb_[:, ps_], Saugb[PR(h), h // 2, 0:128], qk[PR(h), h // 2, cs], start=False, stop=True)
                            P.mm(db_[:, ps_], vaug[:, h, 128:256], pm[:, pos, :], start=True, stop=False)
                            P.mm(db_[:, ps_], Saugb[PR(h), h // 2, 128:256], qk[PR(h), h // 2, cs], start=False, stop=True)
                        chk(3.4)
                        thp = PS()
                        for pos, h in enumerate(HO):
                            P.mm(thp[:, pos * 128:(pos + 1) * 128], sel[:, h * 128:(h + 1) * 128], R1[:, cs])
                        for par in range(2):
                            hsl = slice(par * 256, par * 256 + 256)
                            P.act(dmx[:, hsl], denb[par][:, 0:256], AF.Abs)
                        P.tt("dve", dmx, dmx, thp, ALU.max)
                        P.recip(dmx, dmx)
                        for par in range(2):
                            hsl = slice(par * 256, par * 256 + 256)
                            P.tt("dve", hh[:, hsl], numb[par][:, 0:256], dmx[:, hsl], ALU.mult)
                        chk(3.5)
                        P.act(sq, hh, AF.Square)
                        accn = PS()
                        P.mm(accn, onesb, sq)
                        P.act(sd, accn, AF.Sqrt, bias=eps128, scale=1.0 / 128)
                        P.recip(sd, sd)
                        P.tt("dve", hh, hh, sd, ALU.mult)
                        for pos, h in enumerate(HO):
                            P.stt(ya[:, h, cs], hh[:, pos * 128:(pos + 1) * 128], pc(l, "mng", h), ya[:, h, cs], ALU.mult, ALU.mult)
                        chk(3.6)
                        ups = [PS(), PS()]
                        for h in range(4):
                            P.mm(ups[h // 2][:, (h % 2) * 256:(h % 2) * 256 + 256], ktok[:, (h // 2) * 128:(h // 2) * 128 + 128], vaug[:, h, :])
                        for h in range(4):
                            P.tt("dve", Saug[PR(h), h // 2, :], Saug[PR(h), h // 2, :],
                                 ups[h // 2][PR(h), (h % 2) * 256:(h % 2) * 256 + 256], ALU.add)
                        if c + 1 < NC:
                            for h in range(4):
                                P.ts("dve", Saugb[PR(h), h // 2, :], Saug[PR(h), h // 2, :], decb[PR(h), h, c + 1:c + 2], None, ALU.mult)
                            for h in range(4):
                                P.ts("dve", Saug[PR(h), h // 2, :], Saug[PR(h), h // 2, :], decb[PR(h), h, c + 1:c + 2], None, ALU.mult)

                    chk(4)
                    phase()
                    st["ar"] = YRES
                    vtok = AR([NTT, 512], BF16)
                    w = W()
                    for t_ in range(NTT):
                        acc = PS()
                        n, c0 = divmod(t_ * 128, TN)
                        for k in range(KD):
                            P.mm(acc, hT[n][:, k, c0:c0 + 128], w[:, k, :], start=(k == 0), stop=(k == KD - 1))
                        P.tt("dve", vtok[:, t_, :], acc, vb[:, 1, :], ALU.add)
                    w = W()
                    for j in range(4):
                        for n in range(NT):
                            acc = proj_fm(w, j, n)
                            P.act(yb[:, j, NS(n)], acc[:, 0:TN], AF.Silu, bias=pc(l, "bin_b", 12 + j))
                    sg = AR([TU], F32)
                    ff = sg
                    gsq = AR([TN], BF16)
                    gsd = AR([TN], F32)
                    G = AR([TU], F32)
                    qq = AR([TU], F32)
                    kk = AR([TU], F32)
                    Dt = AR([TU], F32)
                    Et = AR([TU], F32)
                    QK4 = AR([4, TU], BF16)
                    dSt = AR([NCH], F32)
                    oT = AR([TU], F32)
                    am2 = [AR([128], BF16), AR([128], BF16)]
                    khtok2 = [AR([128], BF16), AR([128], BF16)]
                    G3 = G.re("p (n l) -> p n l", l=64)
                    D3 = Dt.re("p (n l) -> p n l", l=64)
                    E3 = Et.re("p (n l) -> p n l", l=64)
                    for h in range(4):
                        wf = W()
                        wq = W(True)
                        for n in range(NT):
                            acc = proj_fm(wf, 0, n)
                            P.act(sg[:, NS(n)], acc[:, 0:TN], AF.Sigmoid, bias=pc(l, "bin_b", 0 + h))
                            acc = proj_fm(wq, 0, n)
                            P.act(qq[:, NS(n)], acc[:, 0:TN], AF.Identity, bias=pc(l, "bin_b", 4 + h))
                        P.ts("pool", kk, sg, nomlt[:, l, h:h + 1], omlt[:, l, h:h + 1], ALU.mult, ALU.add)
                        P.ts("dve", ff, sg, omlt[:, l, h:h + 1], lbt[:, l, h:h + 1], ALU.mult, ALU.add)
                        P.ts("dve", ff, ff, 1e-30, None, ALU.max)
                        P.act(ff, ff, AF.Ln)
                        P.scan(G, ones1.bc([128, TU]), ff, zero1[:, 0:1], ALU.mult, ALU.add)
                        P.tt("dve", D3, G3, blast(G3[:, :, 31], 64), ALU.subtract)
                        P.act(Et, Dt, AF.Exp)
                        P.stt(QK4[:, 0, :], qq, HSC, Et, ALU.mult, ALU.mult)
                        P.act(Et, Dt, AF.Exp, scale=-1.0)
                        P.tt("dve", QK4[:, 1, :], kk, Et, ALU.mult)
                        P.copy("dve", D3[:, 0, :], G3[:, 0, :])
                        if NCH > 1:
                            P.tt("dve", D3[:, 1:NCH, :], G3[:, 1:NCH, :], blast(G3[:, 0:NCH - 1, 63], 64), ALU.subtract)
                        P.act(Et, Dt, AF.Exp)
                        P.stt(QK4[:, 2, :], qq, HSC, Et, ALU.mult, ALU.mult)
                        P.copy("dve", dSt, E3[:, :, 63])
                        P.tt("dve", D3, blast(G3[:, :, 63], 64), G3, ALU.subtract)
                        P.act(Et, Dt, AF.Exp)
                        P.tt("dve", QK4[:, 3, :], kk, Et, ALU.mult)
                        vs = slice(h * 128, (h + 1) * 128)

                        def front(p):
                            ps_ = slice(p * 128, (p + 1) * 128)
                            a = PS()
                            P.mm(a[:, 0:128], QK4[:, 1, ps_], QK4[:, 0, ps_])
                            P.tt("dve", am2[p % 2], a[:, 0:128], maskbd, ALU.mult)
                            ktp = PS(dt=BF16)
                            P.tr(ktp[:, 0:128], QK4[:, 3, ps_], identb)
                            P.copy("act", khtok2[p % 2], ktp[:, 0:128])
                        front(0)
                        for p in range(NTT):
                            ps_ = slice(p * 128, (p + 1) * 128)
                            if p + 1 < NTT:
                                front(p + 1)
                            ups = []
                            for hf in range(2):
                                rows = slice(64 * hf, 64 * hf + 64)
                                up = PS()
                                P.mm(up[:, 0:128], khtok2[p % 2][rows, :], vtok[rows, p, vs])
                                ups.append(up)
                            o = PS()
                            P.mm(o[:, 0:128], vtok[:, p, vs], am2[p % 2], start=True, stop=False)
                            for hf in range(2):
                                cols = slice(p * 128 + 64 * hf, p * 128 + 64 * hf + 64)
                                k_ = hc[h]
                                dS_ = dSt[:, 2 * p + hf:2 * p + hf + 1]
                                P.mm(o[:, 64 * hf:64 * hf + 64], Shbr[h][k_ % 3], QK4[:, 2, cols], start=False, stop=(hf == 1))
                                P.stt(Shbr[h][(k_ + 1) % 3], Sh[:, h, :], dS_, ups[hf][:, 0:128], ALU.mult, ALU.add)
                                P.stt(Sh[:, h, :], Sh[:, h, :], dS_, ups[hf][:, 0:128], ALU.mult, ALU.add)
                                hc[h] = k_ + 1
                            P.copy("act", oT[:, ps_], o[:, 0:128])
                        for n in range(NT):
                            group_norm_gate(oT[:, NS(n)], pc(l, "hng", h), yb[:, h, NS(n)], TN, gsq, gsd)

                    chk(5)
                    phase()
                    st["ar"] = YRES
                    up_ = AR([4, CONVK - 1 + TU], BF16)
                    cv = AR([4, TU], F32)
                    mark_c = st["ar"]
                    sgb = AR([4, TU], BF16)
                    w = W()
                    for j in range(4):
                        for n in range(NT):
                            acc = proj_fm(w, j, n)
                            P.act(sgb[:, j, NS(n)], acc[:, 0:TN], AF.Sigmoid, bias=pc(l, "bin_b", 20 + j))
                    w = W()
                    P.copy("pool", up_[:, :, 0:CONVK - 1], upad)
                    for j in range(4):
                        for n in range(NT):
                            acc = proj_fm(w, j, n)
                            P.stt(up_[:, j, CONVK - 1 + n * TN:CONVK - 1 + (n + 1) * TN], acc[:, 0:TN], pc(l, "bin_b", 16 + j),
                                  sgb[:, j, NS(n)], ALU.add, ALU.mult)
                    P.copy("pool", upad, up_[:, :, TU:TU + CONVK - 1])
                    chk(5.1)
                    P.fence()
                    st["ar"] = mark_c

                    def ev_c(j, n, acc):
                        P.act(cv[:, j, NS(n)], acc[:, 0:TN], AF.Identity, bias=pc(l, "cb", j))
                    conv_fm(ev_c, up_, CONVK, "cw", l, [[AR([128], BF16) for _ in range(CONVK)] for _ in range(2)])
                    chk(5.2)
                    cbt = [AR([TN], BF16), AR([TN], BF16)]
                    sqt = [AR([TN], BF16), AR([TN], BF16)]
                    mu = AR([TN], F32)
                    m2 = AR([TN], F32)
                    var = AR([TN], F32)
                    t1 = AR([TN], F32)
                    for n in range(NT):
                        mean = PS()
                        msq = PS()
                        for j in range(4):
                            P.copy("dve", cbt[j % 2], cv[:, j, NS(n)])
                            P.mm(mean[:, 0:TN], onesb, cbt[j % 2], start=(j == 0), stop=(j == 3))
                            P.act(sqt[j % 2], cv[:, j, NS(n)], AF.Square)
                            P.mm(msq[:, 0:TN], onesb, sqt[j % 2], start=(j == 0), stop=(j == 3))
                        P.act(mu, mean[:, 0:TN], AF.Identity, scale=1.0 / 512)
                        P.act(m2, mean[:, 0:TN], AF.Square, scale=1.0 / 512)
                        P.stt(var, msq[:, 0:TN], 1.0 / 512, m2, ALU.mult, ALU.subtract)
                        P.act(var, var, AF.Sqrt, bias=eps128)
                        P.recip(var, var)
                        chk(5.3)
                        for j in range(4):
                            P.tt("dve", t1, cv[:, j, NS(n)], mu, ALU.subtract)
                            P.tt("dve", t1, t1, var, ALU.mult)
                            P.act(yc[:, j, NS(n)], t1, AF.Silu, bias=pc(l, "lnb", j), scale=pc(l, "lng", j))

                    chk(6)
                    phase()
                    st["ar"] = YRES
                    mg = AR([KD, TU], BF16)
                    gts = [AR([TN], F32) for _ in range(3)]
                    tmp = AR([TN], F32)
                    tmp2 = AR([TN], F32)
                    ys = (ya, yb, yc)
                    for m in range(KD):
                        gws = [W(), W(True), W(True)]
                        bws = [W(True), W(True), W(True)]
                        for n in range(NT):
                            for g in range(3):
                                acc = proj_fm(gws[g], 0, n)
                                P.act(gts[g], acc[:, 0:TN], AF.Sigmoid, bias=pc(l, "bin_b", 24 + g * KD + m))
                            accs = []
                            for g in range(3):
                                acc = PS()
                                for k in range(4):
                                    P.mm(acc[:, 0:TN], bws[g][:, k, :], ys[g][:, k, NS(n)], start=(k == 0), stop=(k == 3))
                                accs.append(acc)
                            P.tt("dve", tmp, gts[0], accs[0][:, 0:TN], ALU.mult)
                            P.tt("dve", tmp2, gts[1], accs[1][:, 0:TN], ALU.mult)
                            P.tt("dve", tmp, tmp, tmp2, ALU.add)
                            P.tt("dve", tmp2, gts[2], accs[2][:, 0:TN], ALU.mult)
                            P.tt("dve", mg[:, m, NS(n)], tmp, tmp2, ALU.add)
                    for c0 in range(0, D, 512):
                        ncol = min(512, D - c0)
                        w = W()
                        for mi in range(ncol // 128):
                            m = c0 // 128 + mi
                            for n in range(NT):
                                acc = PS()
                                for k in range(KD):
                                    P.mm(acc[:, 0:TN], w[:, k, mi * 128:(mi + 1) * 128], mg[:, k, NS(n)], start=(k == 0), stop=(k == KD - 1))
                                P.tt("dve", xT[m][u][:, NS(n)], xT[m][u][:, NS(n)], acc[:, 0:TN], ALU.add)

                    chk(7)
                    phase()

                    def mk_h2(n, sd):
                        for m in range(KD):
                            P.stt(hT[n][:, m, :], xT[m][u][:, NS(n)], pc(l, "nffn", m), sd, ALU.mult, ALU.mult)
                    rmsnorm_to(mk_h2, xT, None, u)
                    actT = AR([KF, TU], BF16)
                    sgt = [AR([TN], F32), AR([TN], F32)]
                    for j0 in range(0, KF, 2):
                        wg = W()
                        wu = W(True)
                        for n in range(NT):
                            for jj in range(2):
                                accg = proj_fm(wg, jj, n)
                                accu = proj_fm(wu, jj, n)
                                P.act(sgt[jj], accg[:, 0:TN], AF.Silu)
                                P.tt("dve", actT[:, j0 + jj, NS(n)], sgt[jj], accu[:, 0:TN], ALU.mult)
                    for c0 in range(0, D, 256):
                        wv = [W(), W(True)]
                        accs = {(mi, n): PS() for mi in range(2) for n in range(NT)}
                        for hf in range(2):
                            for k_ in range(KFH):
                                j = hf * KFH + k_
                                for mi in range(2):
                                    for n in range(NT):
                                        P.mm(accs[mi, n][:, 0:TN], wv[hf][:, k_, mi * 128:(mi + 1) * 128], actT[:, j, NS(n)],
                                             start=(j == 0), stop=(j == KF - 1))
                        for mi in range(2):
                            m = c0 // 128 + mi
                            for n in range(NT):
                                P.tt("dve", xT[m][u][:, NS(n)], xT[m][u][:, NS(n)], accs[mi, n][:, 0:TN], ALU.add)
                    chk(8)

        except StopBuild:
            pass

        phase()
        for u in range(NU if not DBG & 4 else 0):
            def mk_o(n, sd):
                for m in range(KD):
                    P.stt(xT[m][u][:, NS(n)], xT[m][u][:, NS(n)], pcols[:, DEPTH * RPL + m:DEPTH * RPL + m + 1], sd,
                          ALU.mult, ALU.mult)
            rmsnorm_to(mk_o, xT, None, u)
        ostg = [AR([D], F32), AR([D], F32)]
        for tt_ in range(TSEQ // 128):
            u_, c0 = divmod(tt_ * 128, TU)
            stg = ostg[tt_ % 2]
            if DBG & 16:
                P.memset("dve", stg, 1.0)
            for m0 in range(0, KD if not DBG & 16 else 0, 4):
                pt = PS()
                mm_ = min(4, KD - m0)
                for i in range(mm_):
                    P.tr(pt[:, i * 128:(i + 1) * 128], xT[m0 + i][u_][:, c0:c0 + 128], identf)
                P.copy("act" if (m0 // 4) % 2 else "dve", stg[:, m0 * 128:(m0 + mm_) * 128], pt[:, 0:mm_ * 128])
            ob = Buf("out")
            out_bufs.append(ob)
            P.dma("sp", T(out_d[s, tt_ * 128:(tt_ + 1) * 128, :], ob), stg, "xout%d" % (tt_ % 2), arena=True)

    P.add("sp", lambda e: None, reads=out_bufs)
    P.emit_all(stack)
    stack.close()
    return nc, stack


CFG = dict(D=1024, T=2048, TU=1024, DEPTH=4, DFF=2816, NSEQ=2, ARENA=38 * 1024)
_cache = {}


def kernel(**inp):
    cfg = CFG
    inp = {k: np.ascontiguousarray(np.asarray(v, dtype=np.float32)) for k, v in inp.items()}
    ncores = 8
    if "nc" not in _cache:
        _cache["nc"] = build(cfg)
    nc, stack = _cache["nc"]
    params = pack_params(cfg, inp)
    consts = make_consts()
    shared = {k: inp[k] for k in ("w_in", "b_in", "w_branch_a", "w_branch_b", "w_branch_c", "w_out",
                                  "w_ffn_in", "w_ffn_out")}
    shared["params"] = params
    shared["consts"] = consts
    x = inp["x"]
    nseq = cfg["NSEQ"]
    in_maps = [dict(shared, x=np.ascontiguousarray(x[c * nseq:(c + 1) * nseq])) for c in range(ncores)]
    res = run_bass_kernel_spmd(nc, in_maps, core_ids=list(range(ncores)))
    return np.concatenate([r["out"] for r in res.results], axis=0).astype(np.float32)
```

```python
import numpy as np
from contextlib import ExitStack
import concourse.bass as bass
import concourse.mybir as mybir
from concourse.bass_utils import run_bass_kernel_spmd

F32 = mybir.dt.float32
BF16 = mybir.dt.bfloat16
AF = mybir.ActivationFunctionType
ALU = mybir.AluOpType
EPS = 1e-6
BW = 512
O_QK, O_V, O_O, O_I, O_F, O_HF, O_HQ, O_HI, O_HG, O_CA, O_CB, O_G = (
    0, 512, 1024, 1536, 1540, 1544, 2056, 2568, 3080, 3592, 4104, 4616)
CONVK = 31
SLOT = 1024
NSLOT = 12
PREFETCH = 2


class StopBuild(Exception):
    pass


class Buf:
    __slots__ = ("name", "w", "r", "excl")

    def __init__(self, name="", excl=False):
        self.name = name
        self.w = None
        self.r = []
        self.excl = excl


class Ins:
    __slots__ = ("eng", "idx", "emit", "dma", "deps", "raw", "sig", "val")

    def __init__(self, eng, idx, emit, dma):
        self.eng, self.idx, self.emit, self.dma = eng, idx, emit, dma
        self.deps = ()
        self.raw = ()
        self.sig = False
        self.val = 0


class T:
    __slots__ = ("ap", "bufs")

    def __init__(self, ap, bufs):
        self.ap = ap
        self.bufs = bufs if isinstance(bufs, list) else [bufs]

    def __getitem__(self, idx):
        return T(self.ap[idx], self.bufs)

    def bc(self, shape):
        return T(self.ap.to_broadcast(shape), self.bufs)

    def re(self, pat, **kw):
        return T(self.ap.rearrange(pat, **kw), self.bufs)

    def sub(self, idx, buf):
        return T(self.ap[idx], [buf])


def _bufs(*ts):
    out = []
    for t in ts:
        if isinstance(t, T):
            out.extend(t.bufs)
    return out


def _a(t):
    return t.ap if isinstance(t, T) else t


class Prog:
    ENG = ("pe", "act", "dve", "pool", "sp")

    def __init__(self, nc):
        self.nc = nc
        self.q = {e: [] for e in self.ENG}
        self.fence_deps = []
        self.arena_dmas = []
        self.dma_count = {}

    def add(self, eng, emit, reads=(), writes=(), dma=None, arena=False):
        ins = Ins(eng, len(self.q[eng]), emit, dma)
        deps = set()
        raw = set()
        for b in reads:
            if b.w is not None:
                deps.add(b.w)
                raw.add(b.w)
            if b.excl:
                deps.update(r for r in b.r if r.eng != eng)
        for b in writes:
            if b.w is not None:
                deps.add(b.w)
            deps.update(b.r)
        deps.update(self.fence_deps)
        deps.discard(ins)
        ins.deps = deps
        ins.raw = raw
        for b in reads:
            b.r.append(ins)
        for b in writes:
            b.w = ins
            b.r = []
        if dma is not None:
            self.dma_count[dma] = self.dma_count.get(dma, 0) + 1
            ins.val = 16 * self.dma_count[dma]
            if arena:
                self.arena_dmas.append(ins)
        self.q[eng].append(ins)
        return ins

    def fence(self):
        deps = [self.q[e][-1] for e in self.ENG if self.q[e]]
        deps.extend(self.arena_dmas)
        self.arena_dmas = []
        self.fence_deps = deps

    def mm(self, out, lhsT, rhs, start=True, stop=True):
        o, l, r = out.ap, lhsT.ap, rhs.ap
        self.add("pe", lambda e: e.matmul(o, l, r, start=start, stop=stop),
                 reads=_bufs(lhsT, rhs) + ([] if start else _bufs(out)), writes=_bufs(out))

    def tr(self, out, in_, ident):
        o, i, d = out.ap, in_.ap, ident.ap
        self.add("pe", lambda e: e.transpose(o, i, d), reads=_bufs(in_, ident), writes=_bufs(out))

    def act(self, out, in_, func, bias=None, scale=None, eng="act"):
        kw = {}
        if bias is not None:
            kw["bias"] = _a(bias)
        if scale is not None:
            kw["scale"] = _a(scale)
        o, i = out.ap, in_.ap
        self.add("act", lambda e: e.activation(o, i, func, **kw),
                 reads=_bufs(in_, bias, scale), writes=_bufs(out))

    def tt(self, eng, out, in0, in1, op):
        o, a, b = out.ap, in0.ap, in1.ap
        self.add(eng, lambda e: e.tensor_tensor(o, a, b, op), reads=_bufs(in0, in1), writes=_bufs(out))

    def ts(self, eng, out, in0, s1, s2, op0, op1=None):
        o, a = out.ap, in0.ap
        s1a, s2a = _a(s1), _a(s2)
        if op1 is None:
            self.add(eng, lambda e: e.tensor_scalar(o, a, s1a, None, op0),
                     reads=_bufs(in0, s1), writes=_bufs(out))
        else:
            self.add(eng, lambda e: e.tensor_scalar(o, a, s1a, s2a, op0, op1),
                     reads=_bufs(in0, s1, s2), writes=_bufs(out))

    def stt(self, out, in0, scalar, in1, op0, op1):
        o, a, b = out.ap, in0.ap, in1.ap
        sa = _a(scalar)
        self.add("dve", lambda e: e.scalar_tensor_tensor(o, a, sa, b, op0, op1),
                 reads=_bufs(in0, scalar, in1), writes=_bufs(out))

    def scan(self, out, d0, d1, init, op0, op1):
        o, a, b = out.ap, d0.ap, d1.ap
        ia = _a(init)
        self.add("dve", lambda e: e.tensor_tensor_scan(o, a, b, ia, op0, op1),
                 reads=_bufs(d0, d1, init), writes=_bufs(out))

    def recip(self, out, in_):
        o, i = out.ap, in_.ap
        self.add("dve", lambda e: e.reciprocal(o, i), reads=_bufs(in_), writes=_bufs(out))

    def copy(self, eng, out, in_):
        o, i = out.ap, in_.ap
        if eng == "act":
            self.add("act", lambda e: e.activation(o, i, AF.Identity), reads=_bufs(in_), writes=_bufs(out))
        else:
            self.add(eng, lambda e: e.tensor_copy(o, i), reads=_bufs(in_), writes=_bufs(out))

    def memset(self, eng, out, val):
        o = out.ap
        self.add(eng, lambda e: e.memset(o, val), writes=_bufs(out))

    def dma(self, queue, out, in_, key, arena=False):
        o, i = _a(out), _a(in_)
        return self.add(queue, lambda e: e.dma_start(out=o, in_=i), reads=_bufs(in_),
                        writes=_bufs(out), dma=key, arena=arena)

    def emit_all(self, stack):
        nc = self.nc
        allq = [i for e in self.ENG for i in self.q[e]]
        for ins in allq:
            for d in ins.deps:
                if d.dma is None:
                    if d.eng == ins.eng and (d.eng == "pe" or ins.idx - d.idx > 2):
                        continue
                    d.sig = True
        for e in self.ENG:
            c = 0
            for ins in self.q[e]:
                if ins.dma is None and ins.sig:
                    c += 1
                    ins.val = c
        esem = {e: stack.enter_context(nc.semaphore("sem_" + e)) for e in self.ENG}
        dsem = {k: stack.enter_context(nc.semaphore("dsem_" + k)) for k in self.dma_count}
        engobj = {"pe": "tensor", "act": "scalar", "dve": "vector", "pool": "gpsimd", "sp": "sync"}

        def run(en, e):
            waited = {}
            for ins in self.q[en]:
                need = {}
                for d in ins.deps:
                    if d.dma is not None:
                        k, v = ("d", d.dma), d.val
                    else:
                        if d.eng == en and (en == "pe" or ins.idx - d.idx > 2):
                            continue
                        k, v = ("e", d.eng), d.val
                    if v > need.get(k, 0):
                        need[k] = v
                for k, v in need.items():
                    if waited.get(k, 0) >= v:
                        continue
                    waited[k] = v
                    e.wait_ge(dsem[k[1]] if k[0] == "d" else esem[k[1]], v)
                r = ins.emit(e)
                if r is None:
                    continue
                if ins.dma is not None:
                    r.then_inc(dsem[ins.dma], 16)
                elif ins.sig:
                    r.then_inc(esem[en], 1)

        block = stack.enter_context(nc.Block())

        @block.tensor
        def _(e):
            run("pe", e)

        @block.scalar
        def _(e):
            run("act", e)

        @block.vector
        def _(e):
            run("dve", e)

        @block.gpsimd
        def _(e):
            run("pool", e)

        @block.sync
        def _(e):
            run("sp", e)


def param_layout(cfg):
    D = cfg["D"]
    KD = D // 128
    ent = [("bin_a", 12), ("bin_i", 1), ("bin_f", 1), ("bin_b", (3072 + 3 * D) // 128),
           ("nmix", KD), ("nffn", KD), ("mcw", 16), ("mcb", 4), ("mng", 4), ("lbl", 4),
           ("hng", 4), ("cw", CONVK * 4), ("cb", 4), ("lng", 4), ("lnb", 4)]
    off, o = {}, 0
    for k, n in ent:
        off[k] = o
        o += n
    return off, o


def pack_params(cfg, inp):
    D, L = cfg["D"], cfg["DEPTH"]
    rows = []
    for l in range(L):
        b = inp["b_in"][l]
        pad = np.zeros(124, np.float32)
        rows += [b[0:1536], np.concatenate([b[1536:1540], pad]), np.concatenate([b[1540:1544], pad]),
                 b[1544:], inp["norm_mix_g"][l], inp["norm_ffn_g"][l], inp["mlstm_conv_w"][l].reshape(-1),
                 inp["mlstm_conv_b"][l], inp["mlstm_norm_g"][l], inp["hgrn_lb_logits"][l],
                 inp["hgrn_norm_g"][l], inp["conv_w"][l].reshape(-1), inp["conv_b"][l],
                 inp["conv_ln_g"][l], inp["conv_ln_b"][l]]
    rows.append(inp["final_norm_g"])
    flat = np.concatenate([np.asarray(r, np.float32).reshape(-1) for r in rows])
    n = flat.size // 128
    npad = -(-n // 128) * 128
    out = np.zeros((npad, 128), np.float32)
    out.reshape(-1)[:flat.size] = flat
    return out


def make_consts():
    c = np.zeros((128, 896), np.float32)
    c[:, 0:128] = np.eye(128, dtype=np.float32)
    s = np.arange(128)
    c[:, 128:256] = (s[:, None] <= s[None, :]).astype(np.float32)
    c[:, 256:384] = ((s[:, None] <= s[None, :]) & ((s[:, None] // 64) == (s[None, :] // 64))).astype(np.float32)
    for h in range(4):
        c[h, 384 + h * 128: 384 + (h + 1) * 128] = 1.0
    return c


def build(cfg):
    D, TSEQ, TU, DEPTH, DFF, NSEQ = cfg["D"], cfg["T"], cfg["TU"], cfg["DEPTH"], cfg["DFF"], cfg["NSEQ"]
    KD, KF, NU = D // 128, DFF // 128, TSEQ // TU
    TN = min(512, TU)
    NT, NTT = TU // TN, TU // 128
    DIN = O_G + 3 * D
    poff, RPL = param_layout(cfg)
    RP = -(-(DEPTH * RPL + KD) // 128) * 128
    KFH = KF // 2

    nc = bass.Bass("TRN2", target_bir_lowering=False)
    P = Prog(nc)
    stack = ExitStack()

    def dram(name, shape, kind="ExternalInput"):
        return nc.dram_tensor(name, list(shape), F32, kind=kind).ap()

    x_d = dram("x", [NSEQ, TSEQ, D])
    out_d = dram("out", [NSEQ, TSEQ, D], "ExternalOutput")
    win_d = dram("w_in", [DEPTH, D, DIN])
    bin_d = dram("b_in", [DEPTH, DIN])
    wa_d = dram("w_branch_a", [DEPTH, BW, D])
    wb_d = dram("w_branch_b", [DEPTH, BW, D])
    wc_d = dram("w_branch_c", [DEPTH, BW, D])
    wo_d = dram("w_out", [DEPTH, D, D])
    wfi_d = dram("w_ffn_in", [DEPTH, D, 2 * DFF])
    wfo_d = dram("w_ffn_out", [DEPTH, DFF, D])
    par_d = dram("params", [RP, 128])
    con_d = dram("consts", [128, 896])

    def sb(name, shape, dt):
        return stack.enter_context(nc.sbuf_tensor(name, list(shape), dt))

    def tile(name, shape, dt):
        return T(sb(name, shape, dt)[:], Buf(name))

    xT_t = sb("xT", [128, KD, TSEQ], F32)
    xT = [[T(xT_t[:, m, u * TU:(u + 1) * TU], Buf("x")) for u in range(NU)] for m in range(KD)]
    hT_t = sb("hT", [128, KD, TU], BF16)
    hT = [T(hT_t[:, :, n * TN:(n + 1) * TN], Buf("h")) for n in range(NT)]
    ring_t = sb("ring", [128, NSLOT * SLOT], BF16)
    ring_bufs = [Buf("ring") for _ in range(NSLOT)]
    pcols = tile("pcols", [128, RP], F32)
    identf = tile("identf", [128, 128], F32)
    identb = tile("identb", [128, 128], BF16)
    onesb = tile("onesb", [128, 128], BF16)
    maskc = tile("maskc", [128, 128], BF16)
    maskbd = tile("maskbd", [128, 128], BF16)
    sel = tile("sel", [4, 512], F32)
    lbt = tile("lbt", [128, DEPTH, 4], F32)
    omlt = tile("omlt", [128, DEPTH, 4], F32)
    nomlt = tile("nomlt", [128, DEPTH, 4], F32)
    vb = tile("vb", [128, 2, 512], F32)
    Saug = tile("Saug", [128, 2, 256], F32)
    Saugb = tile("Saugb", [128, 2, 256], BF16)
    Sh = tile("Sh", [128, 4, 128], F32)
    Shb = tile("Shb", [128, 4, 128], BF16)
    Shbr_t = sb("Shbr", [128, 4, 3, 128], BF16)
    Shbr = [[T(Shbr_t[:, h, k, :], Buf("shbr")) for k in range(3)] for h in range(4)]
    hc = [0, 0, 0, 0]
    carry = tile("carry", [4, 4], F32)
    qkpad = tile("qkpad", [128, 4, 4], BF16)
    upad = tile("upad", [128, 4, CONVK - 1], BF16)
    zero1 = tile("zero1", [128, 1], F32)
    AE = cfg["ARENA"]
    arena_t = sb("arena", [128, AE], BF16)
    ps_t = [stack.enter_context(nc.psum_tensor("ps%d" % i, [128, 512], F32)) for i in range(8)]
    ps_bufs = [Buf("ps", excl=True) for _ in range(8)]
    st = {"ps": 0, "ar": 0}

    def PS(shape=None, dt=F32):
        i = st["ps"]
        st["ps"] = (i + 1) % 8
        ap = ps_t[i][:]
        if dt == BF16:
            ap = ap.bitcast(BF16)
        return T(ap, ps_bufs[i])

    def AR(shape, dt, parts=128):
        n = 1
        for s_ in shape:
            n *= s_
        ne = n * (2 if dt == F32 else 1)
        off = st["ar"]
        off += off % 2
        assert off + ne <= AE, ("arena overflow", off, ne, AE)
        st["ar"] = off + ne
        ap = arena_t[:, off:off + ne]
        if dt == F32:
            ap = ap.bitcast(F32)
        if len(shape) == 2:
            ap = ap.rearrange("p (a b) -> p a b", b=shape[1])
        elif len(shape) == 3:
            ap = ap.rearrange("p (a b c) -> p a b c", b=shape[1], c=shape[2])
        if parts != 128:
            ap = ap[0:parts]
        return T(ap, Buf("ar"))

    def phase():
        P.fence()
        st["ar"] = 0

    pieces = []
    rst = {"issued": 0, "ptr": 0, "next": 0, "tiles": {}, "slots": {}, "held": []}

    def piece_list(l):
        pl = []

        def win(c0, ncol):
            pl.append((win_d[l, :, c0:c0 + ncol].rearrange("(k p) c -> p k c", p=128), KD, ncol))
        win(O_QK, 512); win(O_V, 512); win(O_O, 512); win(O_I, 8)
        win(O_HI, 512); win(O_HG, 512)
        for h in range(4):
            win(O_HF + 128 * h, 128); win(O_HQ + 128 * h, 128)
        win(O_CB, 512); win(O_CA, 512)
        for m in range(KD):
            for g in range(3):
                win(O_G + g * D + m * 128, 128)
            for wd in (wa_d, wb_d, wc_d):
                pl.append((wd[l, :, m * 128:(m + 1) * 128].rearrange("(k p) c -> p k c", p=128), 4, 128))
        for c0 in range(0, D, 512):
            ncol = min(512, D - c0)
            pl.append((wo_d[l, :, c0:c0 + ncol].rearrange("(k p) c -> p k c", p=128), KD, ncol))
        for j in range(0, KF, 2):
            pl.append((wfi_d[l, :, j * 128:(j + 2) * 128].rearrange("(k p) c -> p k c", p=128), KD, 256))
            pl.append((wfi_d[l, :, DFF + j * 128:DFF + (j + 2) * 128].rearrange("(k p) c -> p k c", p=128), KD, 256))
        for c0 in range(0, D, 256):
            for hf in range(2):
                pl.append((wfo_d[l, hf * KFH * 128:(hf + 1) * KFH * 128, c0:c0 + 256]
                           .rearrange("(k p) c -> p k c", p=128), KFH, 256))
        return pl

    def ring_issue():
        i = rst["issued"]
        src, kt, ncol = pieces[i]
        ns = -(-(kt * ncol) // SLOT)
        live = list(rst["slots"].values())

        def free(p):
            return p + ns <= NSLOT and all(p + ns <= a or p >= a + n for a, n in live)
        cand = [p for p in list(range(rst["ptr"], NSLOT)) + list(range(0, rst["ptr"])) if free(p)]
        if not cand:
            return False
        p0 = cand[0]
        rst["ptr"] = (p0 + ns) % NSLOT
        rst["slots"][i] = (p0, ns)
        ap = ring_t[:, p0 * SLOT:p0 * SLOT + kt * ncol].rearrange("p (k c) -> p k c", c=ncol)
        t = T(ap, ring_bufs[p0:p0 + ns])
        P.dma("pool", t, src, "ring%d" % p0)
        rst["tiles"][i] = t
        rst["issued"] = i + 1
        return True

    def W(hold=False):
        i = rst["next"]
        if not hold:
            for k in rst["held"]:
                rst["slots"].pop(k)
            rst["held"] = []
        while rst["issued"] < min(len(pieces), i + 1 + PREFETCH):
            if not ring_issue():
                assert rst["issued"] > i, "ring too small for held pieces"
                break
        rst["next"] = i + 1
        rst["held"].append(i)
        return rst["tiles"].pop(i)

    for s_ in range(NSEQ):
        for l in range(DEPTH):
            for u in range(NU):
                pieces.extend(piece_list(l))

    def pc(l, name, j=0, parts=128):
        c = l * RPL + poff[name] + j
        return pcols[0:parts, c:c + 1]

    cst = AR([896], F32)
    P.dma("sp", cst, con_d[:, :], "cst", arena=True)
    P.copy("dve", identf, cst[:, 0:128])
    P.copy("dve", identb, cst[:, 0:128])
    P.copy("dve", maskc, cst[:, 128:256])
    P.copy("dve", maskbd, cst[:, 256:384])
    P.copy("dve", sel, cst[0:4, 384:896])
    P.memset("dve", onesb, 1.0)
    P.memset("dve", zero1, 0.0)
    DBG = cfg.get("DBG", 0)
    for r0 in range(0, RP if not DBG & 2 else 0, 128):
        stg = AR([128], F32)
        P.dma("sp", stg, par_d[r0:r0 + 128, :], "par%d" % (r0 // 128), arena=True)
        pt = PS()
        P.tr(pt[:, 0:128], stg, identf)
        P.copy("act", pcols[:, r0:r0 + 128], pt[:, 0:128])
    ex = AR([DEPTH, 4], F32)
    for l in range(DEPTH if not DBG & 1 else 0):
        P.act(ex[:, l, :], pcols[:, l * RPL + poff["lbl"]:l * RPL + poff["lbl"] + 4], AF.Exp)
    sm = AR([4], F32)
    if DBG & 1:
        P.memset("dve", ex, 1.0)
    P.copy("dve", sm, ex[:, 0, :])
    for l in range(1, DEPTH):
        P.tt("dve", sm, sm, ex[:, l, :], ALU.add)
    P.recip(sm, sm)
    P.memset("dve", lbt[:, 0, :], 0.0)
    for l in range(1, DEPTH):
        pl_ = AR([4], F32)
        P.tt("dve", pl_, ex[:, l, :], sm, ALU.mult)
        P.tt("dve", lbt[:, l, :], lbt[:, l - 1, :], pl_, ALU.add)
    P.ts("dve", omlt, lbt, -1.0, 1.0, ALU.mult, ALU.add)
    P.ts("dve", nomlt, omlt, -1.0, None, ALU.mult)

    def rmsnorm_to(dst_fn, xs, gcol_fn, u):
        for n in range(NT):
            acc = PS()
            for m in range(KD):
                sq = AR([TN], BF16) if False else sqbuf[m % 2]
                P.act(sq, xs[m][u][:, n * TN:(n + 1) * TN], AF.Square)
                P.mm(acc[:, 0:TN], onesb, sq, start=(m == 0), stop=(m == KD - 1))
            sd = rsbuf
            P.act(sd, acc[:, 0:TN], AF.Sqrt, bias=epsD, scale=1.0 / D)
            P.recip(sd, sd)
            dst_fn(n, sd)

    def group_norm_gate(src, gcol, gate_inout, width, sq, sd):
        P.act(sq, src, AF.Square)
        acc = PS()
        P.mm(acc[:, 0:width], onesb, sq)
        P.act(sd, acc[:, 0:width], AF.Sqrt, bias=eps128, scale=1.0 / 128)
        P.recip(sd, sd)
        P.stt(sd, src, gcol, sd, ALU.mult, ALU.mult)
        P.tt("dve", gate_inout, sd, gate_inout, ALU.mult)

    def conv_fm(dst_evac, src_pad, K, wname, l, diags):
        for j in range(4):
            diag = diags[j % len(diags)]
            for k in range(K):
                P.ts("dve", diag[k], identb, pc(l, wname, k * 4 + j), None, ALU.mult)
            if K > 4:
                chk(5.17 + 0.0001 * (1 + 10 * j))
            for n in range(NT):
                acc = PS()
                for k in range(K):
                    P.mm(acc[:, 0:TN], diag[k], src_pad[:, j, n * TN + k:n * TN + k + TN],
                         start=(k == 0), stop=(k == K - 1))
                if K > 4:
                    chk(5.17 + 0.0001 * (2 + 10 * j + 3 * n))
                dst_evac(j, n, acc)
                if K > 4:
                    chk(5.17 + 0.0001 * (3 + 10 * j + 3 * n))

    eps_t = tile("eps_t", [128, 2], F32)
    P.memset("dve", eps_t[:, 0:1], EPS)
    P.memset("dve", eps_t[:, 1:2], EPS)
    epsD = eps_t[:, 0:1]
    eps128 = eps_t[:, 1:2]
    sqbuf = [tile("sqb%d" % i, [128, TN], BF16) for i in range(2)]
    rsbuf = tile("rsbuf", [128, TN], F32)

    out_bufs = []

    ones1 = tile("ones1", [128, 1], F32)
    P.memset("dve", ones1, 1.0)

    def bmid(t, n):
        return T(t.ap.unsqueeze(1).to_broadcast([t.ap.shape[0], n, t.ap.shape[1]]), t.bufs)

    def blast(t, n):
        return T(t.ap.unsqueeze(2).to_broadcast([t.ap.shape[0], t.ap.shape[1], n]), t.bufs)

    def PR(h):
        return slice(64 * (h % 2), 64 * (h % 2) + 64)

    def NS(n):
        return slice(n * TN, (n + 1) * TN)

    def proj_fm(w, j, n, ncol=128):
        acc = PS()
        for k in range(KD):
            P.mm(acc[:, 0:TN], w[:, k, j * ncol:(j + 1) * ncol], hT[n][:, k, :], start=(k == 0), stop=(k == KD - 1))
        return acc

    NC = NTT
    NCH = TU // 64
    HSC = 128.0 ** -0.5

    STOP = cfg.get("STOP", 99)

    def chk(k):
        if STOP <= k:
            raise StopBuild()

    for s in range(NSEQ):
        phase()
        xstg = [AR([D], F32), AR([D], F32)]
        for tt_ in range(TSEQ // 128):
            stg = xstg[tt_ % 2]
            P.dma("sp", stg, x_d[s, tt_ * 128:(tt_ + 1) * 128, :], "xin%d" % (tt_ % 2), arena=True)
            u_, c0 = divmod(tt_ * 128, TU)
            for m0 in range(0, KD if not DBG & 8 else 0, 4):
                pt = PS()
                mm_ = min(4, KD - m0)
                for i in range(mm_):
                    P.tr(pt[:, i * 128:(i + 1) * 128], stg[:, (m0 + i) * 128:(m0 + i + 1) * 128], identf)
                for i in range(mm_ if not DBG & 128 else 0):
                    P.copy("dve" if DBG & 32 else ("act" if i % 2 else "dve"), xT[m0 + i][u_][:, c0:c0 + 128], pt[:, i * 128:(i + 1) * 128])

        try:
            for l in range(DEPTH if STOP > 0 else 0):
                P.memset("dve", Saug, 0.0)
                P.memset("dve", Saugb, 0.0)
                P.memset("dve", Sh, 0.0)
                P.memset("dve", Shb, 0.0)
                for h_ in range(4):
                    hc[h_] = 0
                    P.memset("dve", Shbr[h_][0], 0.0)
                P.memset("dve", carry, 0.0)
                P.memset("dve", qkpad, 0.0)
                P.memset("dve", upad, 0.0)
                P.dma("sp", vb[:, 0, :], bin_d[l:l + 1, O_V:O_V + 512].to_broadcast([128, 512]), "vb0")
                P.dma("sp", vb[:, 1, :], bin_d[l:l + 1, O_HI:O_HI + 512].to_broadcast([128, 512]), "vb1")

                for u in range(NU):
                    phase()
                    ya = AR([4, TU], BF16)
                    yb = AR([4, TU], BF16)
                    yc = AR([4, TU], BF16)
                    YRES = st["ar"]
                    vtokh = AR([NTT, 512], BF16)
                    YRES_H = st["ar"]

                    def mk_h(n, sd):
                        for m in range(KD):
                            P.stt(hT[n][:, m, :], xT[m][u][:, NS(n)], pc(l, "nmix", m), sd, ALU.mult, ALU.mult)
                    rmsnorm_to(mk_h, xT, None, u)
                    chk(1)

                    qk = AR([4, TU], BF16)
                    vtok = AR([NTT, 512], BF16)
                    mark_m = st["ar"]
                    qkp = AR([4, 3 + TU], BF16)
                    P.copy("pool", qkp[:, :, 0:3], qkpad[:, :, 0:3])
                    w = W()
                    for j in range(4):
                        for n in range(NT):
                            acc = proj_fm(w, j, n)
                            P.act(qkp[:, j, 3 + n * TN:3 + (n + 1) * TN], acc[:, 0:TN], AF.Identity, bias=pc(l, "bin_a", j))
                    P.copy("pool", qkpad[:, :, 0:3], qkp[:, :, TU:TU + 3])

                    def ev_qk(j, n, acc):
                        P.act(qk[:, j, NS(n)], acc[:, 0:TN], AF.Silu, bias=pc(l, "mcb", j))
                    conv_fm(ev_qk, qkp, 4, "mcw", l, [[AR([128], BF16) for _ in range(4)] for _ in range(2)])
                    for j in range(2):
                        P.ts("dve", qk[:, j, :], qk[:, j, :], 0.125, None, ALU.mult)
                    P.fence()
                    st["ar"] = mark_m
                    chk(2)
                    w = W()
                    for t_ in range(NTT):
                        acc = PS()
                        n, c0 = divmod(t_ * 128, TN)
                        for k in range(KD):
                            P.mm(acc, hT[n][:, k, c0:c0 + 128], w[:, k, :], start=(k == 0), stop=(k == KD - 1))
                        P.tt("dve", vtok[:, t_, :], acc, vb[:, 0, :], ALU.add)
                    w = W()
                    for j in range(4):
                        for n in range(NT):
                            acc = proj_fm(w, j, n)
                            P.act(ya[:, j, NS(n)], acc[:, 0:TN], AF.Sigmoid, bias=pc(l, "bin_a", 8 + j))
                    w = W()
                    R0 = AR([TU], F32, parts=4)
                    R1 = AR([TU], F32, parts=4)
                    R2 = AR([TU], F32, parts=4)
                    R3 = AR([TU], F32, parts=4)
                    nb = AR([1], F32, parts=4)
                    P.ts("dve", nb, pc(l, "bin_f", 0, 4), -1.0, None, ALU.mult)
                    onesrow = ones1[0:4, :].bc([4, TN])
                    for n in range(NT):
                        sl = NS(n)
                        af = PS()
                        ai = PS()
                        for k in range(KD):
                            P.mm(af[0:4, 0:TN], w[:, k, 4:8], hT[n][:, k, :], start=(k == 0), stop=(k == KD - 1))
                        for k in range(KD):
                            P.mm(ai[0:4, 0:TN], w[:, k, 0:4], hT[n][:, k, :], start=(k == 0), stop=(k == KD - 1))
                        P.act(R0[:, sl], af[0:4, 0:TN], AF.Exp, bias=nb, scale=-1.0)
                        P.act(R0[:, sl], R0[:, sl], AF.Ln, bias=1.0)
                        init = carry[:, 0:1] if n == 0 else R1[:, n * TN - 1:n * TN]
                        P.scan(R1[:, sl], onesrow, R0[:, sl], init, ALU.mult, ALU.subtract)
                        P.stt(R0[:, sl], ai[0:4, 0:TN], pc(l, "bin_i", 0, 4), R1[:, sl], ALU.add, ALU.subtract)
                        initm = carry[:, 1:2] if n == 0 else R2[:, n * TN - 1:n * TN]
                        P.scan(R2[:, sl], R0[:, sl], R0[:, sl], initm, ALU.max, ALU.max)
                    dec = AR([NC], F32, parts=4)
                    P.tt("dve", dec[:, 0:1], carry[:, 1:2], R2[:, 127:128], ALU.subtract)
                    if NC > 1:
                        P.tt("dve", dec[:, 1:NC], R2[:, 127:TU - 128:128], R2[:, 255:TU:128], ALU.subtract)
                    P.act(dec, dec, AF.Exp)
                    P.copy("dve", carry[:, 0:1], R1[:, TU - 1:TU])
                    P.copy("dve", carry[:, 1:2], R2[:, TU - 1:TU])
                    for c in range(NC):
                        cs = slice(c * 128, (c + 1) * 128)
                        me = R2[:, c * 128 + 127:c * 128 + 128]
                        P.ts("dve", R3[:, cs], R0[:, cs], me, None, ALU.subtract)
                        P.ts("dve", R1[:, cs], R1[:, cs], me, -1.0, ALU.add, ALU.mult)
                    P.act(R3, R3, AF.Exp)
                    P.act(R1, R1, AF.Exp)
                    wcol = AR([NC, 4], F32)
                    for c in range(NC):
                        pt = PS()
                        P.tr(pt[:, 0:4], R3[:, c * 128:(c + 1) * 128], identf[0:4, 0:4])
                        P.copy("act", wcol[:, c, :], pt[:, 0:4])
                    decb = AR([4, NC], F32)
                    pt = PS()
                    for h in range(4):
                        P.mm(pt[:, h * NC:(h + 1) * NC], sel[:, h * 128:(h + 1) * 128], dec)
                    P.copy("act", decb.re("p h c -> p (h c)"), pt[:, 0:4 * NC])
                    for h in range(4):
                        P.ts("dve", Saug[PR(h), h // 2, :], Saug[PR(h), h // 2, :], decb[PR(h), h, 0:1], None, ALU.mult)
                    P.copy("act", Saugb, Saug)

                    whi = W()
                    whg = W(True)
                    pm = AR([4, 128], BF16)
                    chk(3)
                    ktok = AR([256], BF16)
                    vaug = AR([4, 256], BF16)
                    dmx = AR([512], F32)
                    hh = AR([512], F32)
                    sq = AR([512], BF16)
                    sd = AR([512], F32)
                    HO = (0, 2, 1, 3)
                    for c in range(NC):
                        cs = slice(c * 128, (c + 1) * 128)
                        scb = [PS(), PS()]
                        for pos, h in enumerate(HO):
                            P.mm(scb[pos // 2][:, (pos % 2) * 128:(pos % 2) * 128 + 128], qk[PR(h), 2 + h // 2, cs], qk[PR(h), h // 2, cs])
                        for par in range(2):
                            P.tt("dve", pm[:, 2 * par:2 * par + 2, :], scb[par][:, 0:256].re("p (g t) -> p g t", g=2),
                                 bmid(maskc, 2), ALU.mult)
                        chk(3.1)
                        kt_ps = PS(dt=BF16)
                        for j in range(2):
                            P.tr(kt_ps[:, j * 128:(j + 1) * 128], qk[:, 2 + j, cs], identb)
                        P.copy("act", ktok, kt_ps[:, 0:256])
                        chk(3.2)
                        wc_ = wcol[:, c, :]
                        P.tt("pool", vaug[:, :, 0:128], vtok[:, c, :].re("p (h v) -> p h v", h=4), blast(wc_, 128), ALU.mult)
                        P.copy("pool", vaug[:, :, 128:256], blast(wc_, 128))
                        chk(3.3)
                        numb = [PS(), PS()]
                        denb = [PS(), PS()]
                        for pos, h in enumerate(HO):
                            ps_ = slice((pos % 2) * 128, (pos % 2) * 128 + 128)
                            nb_, db_ = numb[pos // 2], denb[pos // 2]
                            P.mm(nb_[:, ps_], vaug[:, h, 0:128], pm[:, pos, :], start=True, stop=False)
                            P.mm(nb_[:, ps_], Saugb[PR(h), h // 2, 0:128], qk[PR(h), h // 2, cs], start=False, stop=True)
                            P.mm(db_[:, ps_], vaug[:, h, 128:256], pm[:, pos, :], start=True, stop=False)
                            P.mm(db_[:, ps_], Saugb[PR(h), h // 2, 128:256], qk[PR(h), h // 2, cs], start=False, stop=True)
                        chk(3.4)
                        thp = PS()
                        for pos, h in enumerate(HO):
                            P.mm(thp[:, pos * 128:(pos + 1) * 128], sel[:, h * 128:(h + 1) * 128], R1[:, cs])
                        for par in range(2):
                            hsl = slice(par * 256, par * 256 + 256)
                            P.act(dmx[:, hsl], denb[par][:, 0:256], AF.Abs)
                        P.tt("dve", dmx, dmx, thp, ALU.max)
                        P.recip(dmx, dmx)
                        for par in range(2):
                            hsl = slice(par * 256, par * 256 + 256)
                            P.tt("dve", hh[:, hsl], numb[par][:, 0:256], dmx[:, hsl], ALU.mult)
                        chk(3.5)
                        P.act(sq, hh, AF.Square)
                        accn = PS()
                        P.mm(accn, onesb, sq)
                        P.act(sd, accn, AF.Sqrt, bias=eps128, scale=1.0 / 128)
                        P.recip(sd, sd)
                        P.tt("dve", hh, hh, sd, ALU.mult)
                        for pos, h in enumerate(HO):
                            P.stt(ya[:, h, cs], hh[:, pos * 128:(pos + 1) * 128], pc(l, "mng", h), ya[:, h, cs], ALU.mult, ALU.mult)
                        chk(3.6)
                        if True:
                            acc = PS()
                            n_, c0_ = divmod(c * 128, TN)
                            for k in range(KD):
                                P.mm(acc, hT[n_][:, k, c0_:c0_ + 128], whi[:, k, :], start=(k == 0), stop=(k == KD - 1))
                            P.tt("dve", vtokh[:, c, :], acc, vb[:, 1, :], ALU.add)
                        for jn in range(c * 4 * NT // NC, (c + 1) * 4 * NT // NC):
                            j_, n_ = divmod(jn, NT)
                            acc = proj_fm(whg, j_, n_)
                            P.act(yb[:, j_, NS(n_)], acc[:, 0:TN], AF.Silu, bias=pc(l, "bin_b", 12 + j_))
                        ups = [PS(), PS()]
                        for h in range(4):
                            P.mm(ups[h // 2][:, (h % 2) * 256:(h % 2) * 256 + 256], ktok[:, (h // 2) * 128:(h // 2) * 128 + 128], vaug[:, h, :])
                        for h in range(4):
                            P.tt("dve", Saug[PR(h), h // 2, :], Saug[PR(h), h // 2, :],
                                 ups[h // 2][PR(h), (h % 2) * 256:(h % 2) * 256 + 256], ALU.add)
                        if c + 1 < NC:
                            for h in range(4):
                                P.ts("dve", Saugb[PR(h), h // 2, :], Saug[PR(h), h // 2, :], decb[PR(h), h, c + 1:c + 2], None, ALU.mult)
                            for h in range(4):
                                P.ts("dve", Saug[PR(h), h // 2, :], Saug[PR(h), h // 2, :], decb[PR(h), h, c + 1:c + 2], None, ALU.mult)

                    chk(4)
                    phase()
                    st["ar"] = YRES_H
                    vtok = vtokh
                    sg = AR([TU], F32)
                    ff = sg
                    gsq = AR([TN], BF16)
                    gsd = AR([TN], F32)
                    G = AR([TU], F32)
                    qq = AR([TU], F32)
                    kk = AR([TU], F32)
                    Dt = AR([TU], F32)
                    Et = AR([TU], F32)
                    QK4 = AR([4, TU], BF16)
                    dSt = AR([NCH], F32)
                    oT = AR([TU], F32)
                    am2 = [AR([128], BF16), AR([128], BF16)]
                    khtok2 = [AR([128], BF16), AR([128], BF16)]
                    G3 = G.re("p (n l) -> p n l", l=64)
                    D3 = Dt.re("p (n l) -> p n l", l=64)
                    E3 = Et.re("p (n l) -> p n l", l=64)
                    for h in range(4):
                        wf = W()
                        wq = W(True)
                        for n in range(NT):
                            acc = proj_fm(wf, 0, n)
                            P.act(sg[:, NS(n)], acc[:, 0:TN], AF.Sigmoid, bias=pc(l, "bin_b", 0 + h))
                            acc = proj_fm(wq, 0, n)
                            P.act(qq[:, NS(n)], acc[:, 0:TN], AF.Identity, bias=pc(l, "bin_b", 4 + h))
                        P.ts("pool", kk, sg, nomlt[:, l, h:h + 1], omlt[:, l, h:h + 1], ALU.mult, ALU.add)
                        P.ts("dve", ff, sg, omlt[:, l, h:h + 1], lbt[:, l, h:h + 1], ALU.mult, ALU.add)
                        P.ts("dve", ff, ff, 1e-30, None, ALU.max)
                        P.act(ff, ff, AF.Ln)
                        P.scan(G, ones1.bc([128, TU]), ff, zero1[:, 0:1], ALU.mult, ALU.add)
                        P.tt("dve", D3, G3, blast(G3[:, :, 31], 64), ALU.subtract)
                        P.act(Et, Dt, AF.Exp)
                        P.stt(QK4[:, 0, :], qq, HSC, Et, ALU.mult, ALU.mult)
                        P.act(Et, Dt, AF.Exp, scale=-1.0)
                        P.tt("dve", QK4[:, 1, :], kk, Et, ALU.mult)
                        P.copy("dve", D3[:, 0, :], G3[:, 0, :])
                        if NCH > 1:
                            P.tt("dve", D3[:, 1:NCH, :], G3[:, 1:NCH, :], blast(G3[:, 0:NCH - 1, 63], 64), ALU.subtract)
                        P.act(Et, Dt, AF.Exp)
                        P.stt(QK4[:, 2, :], qq, HSC, Et, ALU.mult, ALU.mult)
                        P.copy("dve", dSt, E3[:, :, 63])
                        P.tt("dve", D3, blast(G3[:, :, 63], 64), G3, ALU.subtract)
                        P.act(Et, Dt, AF.Exp)
                        P.tt("dve", QK4[:, 3, :], kk, Et, ALU.mult)
                        vs = slice(h * 128, (h + 1) * 128)

                        def front(p):
                            ps_ = slice(p * 128, (p + 1) * 128)
                            a = PS()
                            P.mm(a[:, 0:128], QK4[:, 1, ps_], QK4[:, 0, ps_])
                            P.tt("dve", am2[p % 2], a[:, 0:128], maskbd, ALU.mult)
                            ktp = PS(dt=BF16)
                            P.tr(ktp[:, 0:128], QK4[:, 3, ps_], identb)
                            P.copy("act", khtok2[p % 2], ktp[:, 0:128])
                        front(0)
                        for p in range(NTT):
                            ps_ = slice(p * 128, (p + 1) * 128)
                            if p + 1 < NTT:
                                front(p + 1)
                            ups = []
                            for hf in range(2):
                                rows = slice(64 * hf, 64 * hf + 64)
                                up = PS()
                                P.mm(up[:, 0:128], khtok2[p % 2][rows, :], vtok[rows, p, vs])
                                ups.append(up)
                            o = PS()
                            P.mm(o[:, 0:128], vtok[:, p, vs], am2[p % 2], start=True, stop=False)
                            for hf in range(2):
                                cols = slice(p * 128 + 64 * hf, p * 128 + 64 * hf + 64)
                                k_ = hc[h]
                                dS_ = dSt[:, 2 * p + hf:2 * p + hf + 1]
                                P.mm(o[:, 64 * hf:64 * hf + 64], Shbr[h][k_ % 3], QK4[:, 2, cols], start=False, stop=(hf == 1))
                                P.stt(Shbr[h][(k_ + 1) % 3], Sh[:, h, :], dS_, ups[hf][:, 0:128], ALU.mult, ALU.add)
                                P.stt(Sh[:, h, :], Sh[:, h, :], dS_, ups[hf][:, 0:128], ALU.mult, ALU.add)
                                hc[h] = k_ + 1
                            P.copy("act", oT[:, ps_], o[:, 0:128])
                        for n in range(NT):
                            group_norm_gate(oT[:, NS(n)], pc(l, "hng", h), yb[:, h, NS(n)], TN, gsq, gsd)

                    chk(5)
                    phase()
                    st["ar"] = YRES
                    up_ = AR([4, CONVK - 1 + TU], BF16)
                    cv = AR([4, TU], F32)
                    mark_c = st["ar"]
                    sgb = AR([4, TU], BF16)
                    w = W()
                    for j in range(4):
                        for n in range(NT):
                            acc = proj_fm(w, j, n)
                            P.act(sgb[:, j, NS(n)], acc[:, 0:TN], AF.Sigmoid, bias=pc(l, "bin_b", 20 + j))
                    w = W()
                    P.copy("pool", up_[:, :, 0:CONVK - 1], upad)
                    for j in range(4):
                        for n in range(NT):
                            acc = proj_fm(w, j, n)
                            P.stt(up_[:, j, CONVK - 1 + n * TN:CONVK - 1 + (n + 1) * TN], acc[:, 0:TN], pc(l, "bin_b", 16 + j),
                                  sgb[:, j, NS(n)], ALU.add, ALU.mult)
                    P.copy("pool", upad, up_[:, :, TU:TU + CONVK - 1])
                    chk(5.1)
                    P.fence()
                    st["ar"] = mark_c

                    def ev_c(j, n, acc):
                        P.act(cv[:, j, NS(n)], acc[:, 0:TN], AF.Identity, bias=pc(l, "cb", j))
                    conv_fm(ev_c, up_, CONVK, "cw", l, [[AR([128], BF16) for _ in range(CONVK)] for _ in range(2)])
                    chk(5.2)
                    cbt = [AR([TN], BF16), AR([TN], BF16)]
                    sqt = [AR([TN], BF16), AR([TN], BF16)]
                    mu = AR([TN], F32)
                    m2 = AR([TN], F32)
                    var = AR([TN], F32)
                    t1 = AR([TN], F32)
                    for n in range(NT):
                        mean = PS()
                        msq = PS()
                        for j in range(4):
                            P.copy("dve", cbt[j % 2], cv[:, j, NS(n)])
                            P.mm(mean[:, 0:TN], onesb, cbt[j % 2], start=(j == 0), stop=(j == 3))
                            P.act(sqt[j % 2], cv[:, j, NS(n)], AF.Square)
                            P.mm(msq[:, 0:TN], onesb, sqt[j % 2], start=(j == 0), stop=(j == 3))
                        P.act(mu, mean[:, 0:TN], AF.Identity, scale=1.0 / 512)
                        P.act(m2, mean[:, 0:TN], AF.Square, scale=1.0 / 512)
                        P.stt(var, msq[:, 0:TN], 1.0 / 512, m2, ALU.mult, ALU.subtract)
                        P.act(var, var, AF.Sqrt, bias=eps128)
                        P.recip(var, var)
                        chk(5.3)
                        for j in range(4):
                            P.tt("dve", t1, cv[:, j, NS(n)], mu, ALU.subtract)
                            P.tt("dve", t1, t1, var, ALU.mult)
                            P.act(yc[:, j, NS(n)], t1, AF.Silu, bias=pc(l, "lnb", j), scale=pc(l, "lng", j))

                    chk(6)
                    phase()
                    st["ar"] = YRES
                    mg = AR([KD, TU], BF16)
                    gts = [AR([TN], F32) for _ in range(3)]
                    tmp = AR([TN], F32)
                    tmp2 = AR([TN], F32)
                    ys = (ya, yb, yc)
                    for m in range(KD):
                        gws = [W(), W(True), W(True)]
                        bws = [W(True), W(True), W(True)]
                        for n in range(NT):
                            for g in range(3):
                                acc = proj_fm(gws[g], 0, n)
                                P.act(gts[g], acc[:, 0:TN], AF.Sigmoid, bias=pc(l, "bin_b", 24 + g * KD + m))
                            accs = []
                            for g in range(3):
                                acc = PS()
                                for k in range(4):
                                    P.mm(acc[:, 0:TN], bws[g][:, k, :], ys[g][:, k, NS(n)], start=(k == 0), stop=(k == 3))
                                accs.append(acc)
                            P.tt("dve", tmp, gts[0], accs[0][:, 0:TN], ALU.mult)
                            P.tt("dve", tmp2, gts[1], accs[1][:, 0:TN], ALU.mult)
                            P.tt("dve", tmp, tmp, tmp2, ALU.add)
                            P.tt("dve", tmp2, gts[2], accs[2][:, 0:TN], ALU.mult)
                            P.tt("dve", mg[:, m, NS(n)], tmp, tmp2, ALU.add)
                    for c0 in range(0, D, 512):
                        ncol = min(512, D - c0)
                        w = W()
                        for mi in range(ncol // 128):
                            m = c0 // 128 + mi
                            for n in range(NT):
                                acc = PS()
                                for k in range(KD):
                                    P.mm(acc[:, 0:TN], w[:, k, mi * 128:(mi + 1) * 128], mg[:, k, NS(n)], start=(k == 0), stop=(k == KD - 1))
                                P.tt("dve", xT[m][u][:, NS(n)], xT[m][u][:, NS(n)], acc[:, 0:TN], ALU.add)

                    chk(7)
                    phase()

                    def mk_h2(n, sd):
                        for m in range(KD):
                            P.stt(hT[n][:, m, :], xT[m][u][:, NS(n)], pc(l, "nffn", m), sd, ALU.mult, ALU.mult)
                    rmsnorm_to(mk_h2, xT, None, u)
                    actT = AR([KF, TU], BF16)
                    sgt = [AR([TN], F32), AR([TN], F32)]
                    for j0 in range(0, KF, 2):
                        wg = W()
                        wu = W(True)
                        for n in range(NT):
                            for jj in range(2):
                                accg = proj_fm(wg, jj, n)
                                accu = proj_fm(wu, jj, n)
                                P.act(sgt[jj], accg[:, 0:TN], AF.Silu)
                                P.tt("dve", actT[:, j0 + jj, NS(n)], sgt[jj], accu[:, 0:TN], ALU.mult)
                    for c0 in range(0, D, 256):
                        wv = [W(), W(True)]
                        accs = {(mi, n): PS() for mi in range(2) for n in range(NT)}
                        for hf in range(2):
                            for k_ in range(KFH):
                                j = hf * KFH + k_
                                for mi in range(2):
                                    for n in range(NT):
                                        P.mm(accs[mi, n][:, 0:TN], wv[hf][:, k_, mi * 128:(mi + 1) * 128], actT[:, j, NS(n)],
                                             start=(j == 0), stop=(j == KF - 1))
                        for mi in range(2):
                            m = c0 // 128 + mi
                            for n in range(NT):
                                P.tt("dve", xT[m][u][:, NS(n)], xT[m][u][:, NS(n)], accs[mi, n][:, 0:TN], ALU.add)
                    chk(8)

        except StopBuild:
            pass

        phase()
        for u in range(NU if not DBG & 4 else 0):
            def mk_o(n, sd):
                for m in range(KD):
                    P.stt(xT[m][u][:, NS(n)], xT[m][u][:, NS(n)], pcols[:, DEPTH * RPL + m:DEPTH * RPL + m + 1], sd,
                          ALU.mult, ALU.mult)
            rmsnorm_to(mk_o, xT, None, u)
        ostg = [AR([D], F32), AR([D], F32)]
        for tt_ in range(TSEQ // 128):
            u_, c0 = divmod(tt_ * 128, TU)
            stg = ostg[tt_ % 2]
            if DBG & 16:
                P.memset("dve", stg, 1.0)
            for m0 in range(0, KD if not DBG & 16 else 0, 4):
                pt = PS()
                mm_ = min(4, KD - m0)
                for i in range(mm_):
                    P.tr(pt[:, i * 128:(i + 1) * 128], xT[m0 + i][u_][:, c0:c0 + 128], identf)
                P.copy("act" if (m0 // 4) % 2 else "dve", stg[:, m0 * 128:(m0 + mm_) * 128], pt[:, 0:mm_ * 128])
            ob = Buf("out")
            out_bufs.append(ob)
            P.dma("sp", T(out_d[s, tt_ * 128:(tt_ + 1) * 128, :], ob), stg, "xout%d" % (tt_ % 2), arena=True)

    P.add("sp", lambda e: None, reads=out_bufs)
    P.emit_all(stack)
    stack.close()
    return nc, stack


CFG = dict(D=1024, T=2048, TU=1024, DEPTH=4, DFF=2816, NSEQ=2, ARENA=38 * 1024)
_cache = {}


def kernel(**inp):
    cfg = CFG
    inp = {k: np.ascontiguousarray(np.asarray(v, dtype=np.float32)) for k, v in inp.items()}
    ncores = 8
    if "nc" not in _cache:
        _cache["nc"] = build(cfg)
    nc, stack = _cache["nc"]
    params = pack_params(cfg, inp)
    consts = make_consts()
    shared = {k: inp[k] for k in ("w_in", "b_in", "w_branch_a", "w_branch_b", "w_branch_c", "w_out",
                                  "w_ffn_in", "w_ffn_out")}
    shared["params"] = params
    shared["consts"] = consts
    x = inp["x"]
    nseq = cfg["NSEQ"]
    in_maps = [dict(shared, x=np.ascontiguousarray(x[c * nseq:(c + 1) * nseq])) for c in range(ncores)]
    res = run_bass_kernel_spmd(nc, in_maps, core_ids=list(range(ncores)))
    return np.concatenate([r["out"] for r in res.results], axis=0).astype(np.float32)
```
